# Optimizing a Trainium2 kernel written in Bass

```python
import math
import jax
import jax.numpy as jnp
from jax import lax
import numpy as np


D_MODEL = 1024
BATCH = 8
SEQ = 4096
DEPTH = 4

HEAD_DIM = 64
NSA_HEADS = D_MODEL // (2 * HEAD_DIM)
NSA_KV_HEADS = 2
NSA_GROUP = NSA_HEADS // NSA_KV_HEADS
CMP_LEN = 32
CMP_STRIDE = 16
CMP_HIDDEN = 128
SEL_BLOCK = 64
N_SEL = 16
WINDOW = 512
Q_BLOCK = 128
FORCE_BONUS = 1.0e4
ROPE_THETA = 500000.0
ROPE_DIM = HEAD_DIM // 4

GDN_HEADS = D_MODEL // (4 * HEAD_DIM)
GDN_CONV = 4
GDN_CHUNK = 64

CONF_CH = D_MODEL // 4
CONF_WIDTH = 31

N_GROUPS = 4
EXPERTS_PER_GROUP = 8
N_EXPERTS = N_GROUPS * EXPERTS_PER_GROUP
TOP_K_IN_GROUP = 2
EXPERT_FF = 512
MOE_BLOCK = 256

NSA_WIDTH = NSA_HEADS * HEAD_DIM
KV_WIDTH = NSA_KV_HEADS * HEAD_DIM
GDN_WIDTH = GDN_HEADS * HEAD_DIM
MIX_WIDTH = NSA_WIDTH + GDN_WIDTH + CONF_CH
SPLIT_SIZES = (NSA_WIDTH, 6 * KV_WIDTH, 3 * NSA_HEADS, 3 * GDN_WIDTH, GDN_WIDTH, GDN_HEADS, GDN_HEADS, 2 * CONF_CH)
D_IN = NSA_WIDTH + 6 * KV_WIDTH + 3 * NSA_HEADS + 4 * GDN_WIDTH + 2 * GDN_HEADS + 2 * CONF_CH

DEEPNORM_ALPHA = (2.0 * DEPTH) ** 0.25
DEEPNORM_BETA = (8.0 * DEPTH) ** -0.25

kernel_name = 'hybrid_nsa_gdn_conformer_hmoe'


def _layer_norm(x, w, b, eps=1e-5):
    xf = x.astype(jnp.float32)
    mu = jnp.mean(xf, axis=-1, keepdims=True)
    var = jnp.mean(jnp.square(xf - mu), axis=-1, keepdims=True)
    return ((xf - mu) * lax.rsqrt(var + eps) * w + b).astype(x.dtype)


def _l2norm(x, eps=1e-6):
    xf = x.astype(jnp.float32)
    return xf * lax.rsqrt(jnp.sum(xf * xf, axis=-1, keepdims=True) + eps)


def _rope_tables(seq):
    inv = ROPE_THETA ** (-jnp.arange(0, ROPE_DIM, 2, dtype=jnp.float32) / ROPE_DIM)
    ang = jnp.arange(seq, dtype=jnp.float32)[:, None] * inv[None, :]
    return jnp.cos(ang), jnp.sin(ang)


def _partial_rope(x, cos, sin):
    half = ROPE_DIM // 2
    x1 = x[..., :half].astype(jnp.float32)
    x2 = x[..., half:ROPE_DIM].astype(jnp.float32)
    c = cos[:, None, :]
    s = sin[:, None, :]
    rot = jnp.concatenate([x1 * c - x2 * s, x2 * c + x1 * s], axis=-1).astype(x.dtype)
    return jnp.concatenate([rot, x[..., ROPE_DIM:]], axis=-1)


def _causal_depthwise_conv(x, w):
    k = w.shape[0]
    return lax.conv_general_dilated(
        x, w[:, None, :].astype(x.dtype), window_strides=(1,), padding=((k - 1, 0),),
        dimension_numbers=('NWC', 'WIO', 'NWC'), feature_group_count=x.shape[-1])


def _masked_softmax(s, mask):
    s = jnp.where(mask, s.astype(jnp.float32), -jnp.inf)
    m = jnp.max(s, axis=-1, keepdims=True)
    m = jnp.where(jnp.isfinite(m), m, 0.0)
    e = jnp.exp(s - m)
    return e / jnp.maximum(jnp.sum(e, axis=-1, keepdims=True), 1e-30)


def _compress(t, pe, w1, w2):
    B, S = t.shape[:2]
    n_chunk = S // CMP_STRIDE
    r = CMP_LEN // CMP_STRIDE
    n_cmp = n_chunk - r + 1
    ch = t.reshape(B, n_chunk, CMP_STRIDE, NSA_KV_HEADS, HEAD_DIM)
    blk = jnp.concatenate([ch[:, i:i + n_cmp] for i in range(r)], axis=2)
    blk = blk + pe[None, None, :, None, :]
    blk = blk.transpose(0, 1, 3, 2, 4).reshape(B, n_cmp, NSA_KV_HEADS, CMP_LEN * HEAD_DIM)
    return jax.nn.silu(blk @ w1) @ w2


def _overlap_matrix(n_cmp, n_sb):
    c0 = jnp.arange(n_cmp)[:, None] * CMP_STRIDE
    s0 = jnp.arange(n_sb)[None, :] * SEL_BLOCK
    ov = jnp.minimum(c0 + CMP_LEN, s0 + SEL_BLOCK) - jnp.maximum(c0, s0)
    return (jnp.maximum(ov, 0) / CMP_STRIDE).astype(jnp.float32)


def _nsa(q, k_cmp, v_cmp, k_sel, v_sel, k_win, v_win, gates, cmp_pe, cmp_w1, cmp_w2):
    B, S = q.shape[:2]
    n_sb = S // SEL_BLOCK
    n_sel = min(N_SEL, n_sb)
    n_qb = S // Q_BLOCK
    kc = _compress(k_cmp, cmp_pe[0], cmp_w1[0], cmp_w2[0])
    vc = _compress(v_cmp, cmp_pe[1], cmp_w1[1], cmp_w2[1])
    n_cmp = kc.shape[1]
    cmp_end = jnp.arange(n_cmp) * CMP_STRIDE + (CMP_LEN - 1)
    overlap = _overlap_matrix(n_cmp, n_sb)
    ks_blk = k_sel.reshape(B, n_sb, SEL_BLOCK, NSA_KV_HEADS, HEAD_DIM).transpose(0, 3, 1, 2, 4)
    vs_blk = v_sel.reshape(B, n_sb, SEL_BLOCK, NSA_KV_HEADS, HEAD_DIM).transpose(0, 3, 1, 2, 4)
    pad = ((0, 0), (WINDOW, 0), (0, 0), (0, 0))
    kw_pad = jnp.pad(k_win, pad)
    vw_pad = jnp.pad(v_win, pad)
    b_ix = jnp.arange(B)[:, None, None, None]
    h_ix = jnp.arange(NSA_KV_HEADS)[None, :, None, None]
    blk_ids = jnp.arange(n_sb)
    scale = HEAD_DIM ** -0.5

    def query_block(args):
        qb, gb, s0 = args
        t = s0 + jnp.arange(Q_BLOCK)
        qg = qb.reshape(B, Q_BLOCK, NSA_KV_HEADS, NSA_GROUP, HEAD_DIM) * scale
        sc = jnp.einsum('bqhgd,bchd->bhgqc', qg, kc)
        pc = _masked_softmax(sc, cmp_end[None, :] <= t[:, None])
        o_cmp = jnp.einsum('bhgqc,bchd->bqhgd', pc.astype(vc.dtype), vc)
        imp = jnp.einsum('bhgqc,cj->bhqj', pc, overlap)
        cur = t // SEL_BLOCK
        forced = (blk_ids[None, :] == 0) | (blk_ids[None, :] == cur[:, None]) | (blk_ids[None, :] == cur[:, None] - 1)
        causal_blk = blk_ids[None, :] <= cur[:, None]
        imp = jnp.where(causal_blk, imp + jnp.where(forced, FORCE_BONUS, 0.0), -jnp.inf)
        _, idx = lax.top_k(imp, n_sel)
        valid = idx <= cur[None, None, :, None]
        kg = ks_blk[b_ix, h_ix, idx]
        vg = vs_blk[b_ix, h_ix, idx]
        ss = jnp.einsum('bqhgd,bhqnkd->bhgqnk', qg, kg)
        tok = idx[..., None] * SEL_BLOCK + jnp.arange(SEL_BLOCK)
        smask = valid[..., None] & (tok <= t[None, None, :, None, None])
        ps = _masked_softmax(ss.reshape(B, NSA_KV_HEADS, NSA_GROUP, Q_BLOCK, n_sel * SEL_BLOCK),
                             smask.reshape(B, NSA_KV_HEADS, 1, Q_BLOCK, n_sel * SEL_BLOCK))
        o_sel = jnp.einsum('bhgqnk,bhqnkd->bqhgd', ps.reshape(ss.shape).astype(vg.dtype), vg)
        kw = lax.dynamic_slice_in_dim(kw_pad, s0, Q_BLOCK + WINDOW, axis=1)
        vw = lax.dynamic_slice_in_dim(vw_pad, s0, Q_BLOCK + WINDOW, axis=1)
        p = s0 - WINDOW + jnp.arange(Q_BLOCK + WINDOW)
        wmask = (p[None, :] <= t[:, None]) & (p[None, :] > t[:, None] - WINDOW) & (p[None, :] >= 0)
        sw = jnp.einsum('bqhgd,bkhd->bhgqk', qg, kw)
        pw = _masked_softmax(sw, wmask)
        o_win = jnp.einsum('bhgqk,bkhd->bqhgd', pw.astype(vw.dtype), vw)
        o = jnp.stack([o_cmp, o_sel, o_win], axis=-1).reshape(B, Q_BLOCK, NSA_HEADS, HEAD_DIM, 3)
        return jnp.einsum('bqhdr,bqhr->bqhd', o, gb.astype(o.dtype))

    q_blocks = q.reshape(B, n_qb, Q_BLOCK, NSA_HEADS, HEAD_DIM).swapaxes(0, 1)
    g_blocks = gates.reshape(B, n_qb, Q_BLOCK, NSA_HEADS, 3).swapaxes(0, 1)
    starts = jnp.arange(n_qb) * Q_BLOCK
    out = lax.map(query_block, (q_blocks, g_blocks, starts))
    return out.swapaxes(0, 1).reshape(B, S, NSA_WIDTH).astype(q.dtype)


def _chunked_gated_delta_rule(q, k, v, g, beta):
    B, S, H, Dk = q.shape
    C = GDN_CHUNK
    N = S // C

    def chunks(t):
        t = t.astype(jnp.float32)
        return jnp.moveaxis(t.reshape((B, N, C, H) + t.shape[3:]), 3, 1)

    q, k, v, g, beta = chunks(q), chunks(k), chunks(v), chunks(g), chunks(beta)
    q = q * Dk ** -0.5
    g = jnp.cumsum(g, axis=-1)
    tri = jnp.tril(jnp.ones((C, C), dtype=bool))
    tri_strict = jnp.tril(jnp.ones((C, C), dtype=bool), -1)
    decay = jnp.exp(jnp.where(tri, g[..., :, None] - g[..., None, :], -jnp.inf))
    kb = k * beta[..., None]
    a = jnp.where(tri_strict, jnp.einsum('bhnid,bhnjd->bhnij', kb, k) * decay, 0.0)
    t_mat = jnp.eye(C, dtype=jnp.float32) + a
    u = lax.linalg.triangular_solve(t_mat, v * beta[..., None], left_side=True, lower=True)
    w = lax.linalg.triangular_solve(t_mat, kb * jnp.exp(g)[..., None], left_side=True, lower=True)
    qk = jnp.einsum('bhnid,bhnjd->bhnij', q, k) * decay
    q_dec = q * jnp.exp(g)[..., None]
    g_last = g[..., -1]
    k_dec = k * jnp.exp(g_last[..., None] - g)[..., None]

    def step(state, xs):
        qk_n, q_n, k_n, u_n, w_n, gl_n = xs
        v_new = u_n - jnp.einsum('bhcd,bhde->bhce', w_n, state)
        o = jnp.einsum('bhcd,bhde->bhce', q_n, state) + jnp.einsum('bhij,bhje->bhie', qk_n, v_new)
        state = state * jnp.exp(gl_n)[..., None, None] + jnp.einsum('bhcd,bhce->bhde', k_n, v_new)
        return state, o

    xs = tuple(jnp.moveaxis(t, 2, 0) for t in (qk, q_dec, k_dec, u, w, g_last))
    s0 = jnp.zeros((B, H, Dk, v.shape[-1]), jnp.float32)
    _, o = lax.scan(step, s0, xs)
    return o.transpose(1, 0, 3, 2, 4).reshape(B, S, H, v.shape[-1])


def _gated_deltanet(qkv, z, b, a, conv_w, a_log, dt_bias, norm_w):
    B, S, _ = qkv.shape
    h = jax.nn.silu(_causal_depthwise_conv(qkv, conv_w)).reshape(B, S, 3, GDN_HEADS, HEAD_DIM)
    q = _l2norm(h[:, :, 0])
    k = _l2norm(h[:, :, 1])
    v = h[:, :, 2].astype(jnp.float32)
    beta = jax.nn.sigmoid(b.astype(jnp.float32))
    g = -jnp.exp(a_log.astype(jnp.float32)) * jax.nn.softplus(a.astype(jnp.float32) + dt_bias.astype(jnp.float32))
    o = _chunked_gated_delta_rule(q, k, v, g, beta)
    o = o * lax.rsqrt(jnp.mean(o * o, axis=-1, keepdims=True) + 1e-6) * norm_w.astype(jnp.float32)
    o = o * jax.nn.silu(z.astype(jnp.float32).reshape(B, S, GDN_HEADS, HEAD_DIM))
    return o.reshape(B, S, GDN_WIDTH).astype(qkv.dtype)


def _conformer_conv(h, dw_w, dw_b, ln_w, ln_b):
    val, gate = jnp.split(h, 2, axis=-1)
    u = val * jax.nn.sigmoid(gate)
    u = _causal_depthwise_conv(u, dw_w) + dw_b
    return jax.nn.silu(_layer_norm(u, ln_w, ln_b))


def _hier_moe(x, w_group, b_group, w_expert, b_expert, w_gate, w_up, w_down):
    B, S, D = x.shape
    T = B * S
    K = TOP_K_IN_GROUP
    xt = x.reshape(T, D)
    g_prob = jax.nn.softmax((xt @ w_group + b_group).astype(jnp.float32), axis=-1)
    g_w, g_idx = lax.top_k(g_prob, 1)
    e_logits = (xt @ w_expert + b_expert).astype(jnp.float32).reshape(T, N_GROUPS, EXPERTS_PER_GROUP)
    e_in = jnp.take_along_axis(e_logits, g_idx[:, :, None], axis=1)[:, 0]
    e_val, e_loc = lax.top_k(e_in, K)
    e_w = jax.nn.softmax(e_val, axis=-1) * g_w
    e_id = g_idx * EXPERTS_PER_GROUP + e_loc
    TK = T * K
    flat_id = e_id.reshape(TK)
    order = jnp.argsort(flat_id)
    sorted_id = flat_id[order]
    counts = jnp.bincount(flat_id, length=N_EXPERTS)
    padded = (counts + MOE_BLOCK - 1) // MOE_BLOCK * MOE_BLOCK
    seg_start = jnp.cumsum(counts) - counts
    pad_end = jnp.cumsum(padded)
    pad_start = pad_end - padded
    dest = pad_start[sorted_id] + jnp.arange(TK) - seg_start[sorted_id]
    n_blocks = -(-TK // MOE_BLOCK) + N_EXPERTS
    src_tok = order // K
    buf = jnp.zeros((n_blocks * MOE_BLOCK, D), x.dtype).at[dest].set(xt[src_tok])
    blk_expert = jnp.minimum(jnp.searchsorted(pad_end, jnp.arange(n_blocks) * MOE_BLOCK, side='right'), N_EXPERTS - 1)

    def expert_block(args):
        xb, e = args
        hid = jax.nn.silu(xb @ w_gate[e]) * (xb @ w_up[e])
        return hid @ w_down[e]

    y_buf = lax.map(expert_block, (buf.reshape(n_blocks, MOE_BLOCK, D), blk_expert)).reshape(-1, D)
    y = y_buf[dest] * e_w.reshape(TK)[order][:, None].astype(x.dtype)
    return jnp.zeros((T, D), x.dtype).at[src_tok].add(y).reshape(B, S, D)


def setup_inputs(seed: int = 0) -> dict:
    key = jax.random.key(seed)
    ks = jax.random.split(key, 26)
    L = DEPTH

    def nrm(k, shape, scale):
        return jax.random.normal(k, shape, jnp.float32) * scale

    dt = jnp.exp(jax.random.uniform(ks[8], (L, GDN_HEADS), jnp.float32, math.log(1e-3), math.log(1e-1)))
    return {
        'x': nrm(ks[0], (BATCH, SEQ, D_MODEL), 1.0),
        'w_in': nrm(ks[1], (L, D_MODEL, D_IN), D_MODEL ** -0.5),
        'w_out': nrm(ks[2], (L, MIX_WIDTH, D_MODEL), MIX_WIDTH ** -0.5 * DEEPNORM_BETA),
        'nsa_cmp_pe': nrm(ks[3], (L, 2, CMP_LEN, HEAD_DIM), 0.1),
        'nsa_cmp_w1': nrm(ks[4], (L, 2, CMP_LEN * HEAD_DIM, CMP_HIDDEN), (CMP_LEN * HEAD_DIM) ** -0.5),
        'nsa_cmp_w2': nrm(ks[5], (L, 2, CMP_HIDDEN, HEAD_DIM), CMP_HIDDEN ** -0.5),
        'gdn_conv_w': nrm(ks[6], (L, GDN_CONV, 3 * GDN_WIDTH), GDN_CONV ** -0.5),
        'gdn_a_log': jnp.log(jax.random.uniform(ks[7], (L, GDN_HEADS), jnp.float32, 1.0, 16.0)),
        'gdn_dt_bias': dt + jnp.log(-jnp.expm1(-dt)),
        'gdn_norm_w': 1.0 + nrm(ks[9], (L, HEAD_DIM), 0.02),
        'conf_dw_w': nrm(ks[10], (L, CONF_WIDTH, CONF_CH), CONF_WIDTH ** -0.5),
        'conf_dw_b': nrm(ks[11], (L, CONF_CH), 0.02),
        'conf_ln_w': 1.0 + nrm(ks[12], (L, CONF_CH), 0.02),
        'conf_ln_b': nrm(ks[13], (L, CONF_CH), 0.02),
        'ln1_w': 1.0 + nrm(ks[14], (L, D_MODEL), 0.02),
        'ln1_b': nrm(ks[15], (L, D_MODEL), 0.02),
        'ln2_w': 1.0 + nrm(ks[16], (L, D_MODEL), 0.02),
        'ln2_b': nrm(ks[17], (L, D_MODEL), 0.02),
        'moe_w_group': nrm(ks[18], (L, D_MODEL, N_GROUPS), D_MODEL ** -0.5),
        'moe_b_group': nrm(ks[19], (L, N_GROUPS), 0.01),
        'moe_w_expert': nrm(ks[20], (L, D_MODEL, N_EXPERTS), D_MODEL ** -0.5),
        'moe_b_expert': nrm(ks[21], (L, N_EXPERTS), 0.01),
        'moe_w_gate': nrm(ks[22], (L, N_EXPERTS, D_MODEL, EXPERT_FF), D_MODEL ** -0.5),
        'moe_w_up': nrm(ks[23], (L, N_EXPERTS, D_MODEL, EXPERT_FF), D_MODEL ** -0.5),
        'moe_w_down': nrm(ks[24], (L, N_EXPERTS, EXPERT_FF, D_MODEL), EXPERT_FF ** -0.5 * DEEPNORM_BETA),
    }


def reference(x, w_in, w_out, nsa_cmp_pe, nsa_cmp_w1, nsa_cmp_w2, gdn_conv_w, gdn_a_log, gdn_dt_bias,
              gdn_norm_w, conf_dw_w, conf_dw_b, conf_ln_w, conf_ln_b, ln1_w, ln1_b, ln2_w, ln2_b,
              moe_w_group, moe_b_group, moe_w_expert, moe_b_expert, moe_w_gate, moe_w_up, moe_w_down):
    B, S, _ = x.shape
    cos, sin = _rope_tables(S)
    split_points = [int(p) for p in np.cumsum(SPLIT_SIZES)[:-1]]
    for l in range(DEPTH):
        h = x @ w_in[l]
        q, kv, gate_logits, gdn_qkv, gdn_z, gdn_b, gdn_a, conf_in = jnp.split(h, split_points, axis=-1)
        q = _partial_rope(q.reshape(B, S, NSA_HEADS, HEAD_DIM), cos, sin)
        kv = kv.reshape(B, S, 6, NSA_KV_HEADS, HEAD_DIM)
        k_cmp = _partial_rope(kv[:, :, 0], cos, sin)
        k_sel = _partial_rope(kv[:, :, 2], cos, sin)
        k_win = _partial_rope(kv[:, :, 4], cos, sin)
        gates = jax.nn.sigmoid(gate_logits).reshape(B, S, NSA_HEADS, 3)
        y_nsa = _nsa(q, k_cmp, kv[:, :, 1], k_sel, kv[:, :, 3], k_win, kv[:, :, 5], gates,
                     nsa_cmp_pe[l], nsa_cmp_w1[l], nsa_cmp_w2[l])
        y_gdn = _gated_deltanet(gdn_qkv, gdn_z, gdn_b, gdn_a, gdn_conv_w[l], gdn_a_log[l], gdn_dt_bias[l], gdn_norm_w[l])
        y_conf = _conformer_conv(conf_in, conf_dw_w[l], conf_dw_b[l], conf_ln_w[l], conf_ln_b[l])
        mix = jnp.concatenate([y_nsa, y_gdn, y_conf], axis=-1) @ w_out[l]
        x = _layer_norm(DEEPNORM_ALPHA * x + mix, ln1_w[l], ln1_b[l])
        moe = _hier_moe(x, moe_w_group[l], moe_b_group[l], moe_w_expert[l], moe_b_expert[l],
                        moe_w_gate[l], moe_w_up[l], moe_w_down[l])
        x = _layer_norm(DEEPNORM_ALPHA * x + moe, ln2_w[l], ln2_b[l])
    return x
```

```python
import numpy as np
import concourse.bass as bass
import concourse.mybir as mybir
from concourse.bass_utils import run_bass_kernel_spmd

F32 = mybir.dt.float32
BF16 = mybir.dt.bfloat16
I32 = mybir.dt.int32
U32 = mybir.dt.uint32
AF = mybir.ActivationFunctionType
ALU = mybir.AluOpType
AX = mybir.AxisListType

D = 1024; SEQ = 4096; DEPTH = 4; NT = SEQ // 128
HD = 64; NH = 8; NKV = 2; G = 4
D_IN = 2848
NE = 32; FF = 512; CAP = 512
ALPHA = (2.0 * DEPTH) ** 0.25
EPS = 1e-5


class Buf:
    _n = 0

    def __init__(self, ap, name=None):
        self.ap = ap
        Buf._n += 1
        self.name = name or f"b{Buf._n}"
        self.dsem = None
        self.reset()

    def reset(self):
        self.w = None
        self.r = {}

    def __getitem__(self, idx):
        return self.ap[idx]


class Sched:
    ENG = ("pe", "act", "dve", "pool", "sp")

    def __init__(self, nc, same_sync=True):
        self.nc = nc
        self.e = {"pe": nc.tensor, "act": nc.scalar, "dve": nc.vector, "pool": nc.gpsimd, "sp": nc.sync}
        self.same_sync = same_sync
        self.sem = {k: nc.alloc_semaphore(f"sem_{k}") for k in self.ENG}
        self.dma_sems = []
        self.needed = {k: set() for k in self.ENG}
        self.bufs = []
        self.start_pass(True)

    def start_pass(self, dry):
        self.dry = dry
        self.idx = {k: 0 for k in self.ENG}
        self.val = {k: 0 for k in self.ENG}
        self.idx2val = {k: {} for k in self.ENG}
        self.seen = {k: {} for k in self.ENG}
        self.dma_cnt = {}
        for b in self.bufs:
            b.reset()
        self.ninst = 0

    def buf(self, ap, name=None):
        b = Buf(ap, name)
        self.bufs.append(b)
        return b

    def _wait(self, eng, tok):
        if tok is None:
            return
        if tok[0] == "c":
            _, e2, i2 = tok
            if e2 == eng and (eng == "pe" or not self.same_sync):
                return
            if self.dry:
                self.needed[e2].add(i2)
                return
            v = self.idx2val[e2][i2]
            key = ("c", e2)
            if self.seen[eng].get(key, 0) >= v:
                return
            self.seen[eng][key] = v
            self.e[eng].wait_ge(self.sem[e2], v)
        else:
            _, si, v = tok
            if self.dry:
                return
            key = ("d", si)
            if self.seen[eng].get(key, 0) >= v:
                return
            self.seen[eng][key] = v
            self.e[eng].wait_ge(self.dma_sems[si], v)

    def _deps(self, eng, reads, writes):
        for b in reads:
            self._wait(eng, b.w)
        for b in writes:
            self._wait(eng, b.w)
            for t in b.r.values():
                self._wait(eng, t)

    def op(self, eng, fn, reads=(), writes=()):
        self._deps(eng, reads, writes)
        i = self.idx[eng]
        self.idx[eng] = i + 1
        self.ninst += 1
        tok = ("c", eng, i)
        if not self.dry:
            ins = fn()
            if i in self.needed[eng]:
                self.val[eng] += 1
                ins.then_inc(self.sem[eng], 1)
                self.idx2val[eng][i] = self.val[eng]
        for b in reads:
            b.r[eng] = tok
        for b in writes:
            b.w = tok
            b.r = {}
        return tok

    def dma(self, q, out_ap, in_ap, reads=(), writes=(), fn=None, **kw):
        self._deps(q, reads, writes)
        key = (writes[0] if writes else reads[0])
        if key.dsem is None:
            key.dsem = len(self.dma_sems)
            self.dma_sems.append(self.nc.alloc_semaphore(f"dsem{key.dsem}"))
        si = key.dsem
        self.dma_cnt[si] = self.dma_cnt.get(si, 0) + 16
        tok = ("d", si, self.dma_cnt[si])
        self.ninst += 1
        if not self.dry:
            if fn is not None:
                fn().then_inc(self.dma_sems[si], 16)
            else:
                self.e[q].dma_start(out=out_ap, in_=in_ap, **kw).then_inc(self.dma_sems[si], 16)
        for b in reads:
            b.r[("d", si)] = tok
        for b in writes:
            b.w = tok
            b.r = {}
        return tok

    def barrier(self):
        for eng in self.ENG:
            for e2 in self.ENG:
                if self.idx[e2] > 0:
                    self._wait(eng, ("c", e2, self.idx[e2] - 1))
            for si, cnt in list(self.dma_cnt.items()):
                self._wait(eng, ("d", si, cnt))
        import os as _os
        if not self.dry and _os.environ.get("HWBAR", "0") == "1":
            self.nc.all_engine_barrier()

    def finish(self, eng, bufs):
        for b in bufs:
            self._wait(eng, b.w)
            for t in b.r.values():
                self._wait(eng, t)


class Ctx:
    pass


def host_consts():
    c = {}
    c["ident"] = np.eye(128, dtype=np.float32)
    i = np.arange(128)
    c["ustrict"] = (i[:, None] < i[None, :]).astype(np.float32)
    c["ones"] = np.ones((128, 128), np.float32)
    c["ebase"] = np.tile((np.arange(NE) * CAP).astype(np.float32)[None, :], (128, 1))
    c["tri_incl"] = (i[:, None] <= i[None, :]).astype(np.float32)
    c["negmask"] = np.where(i[None, :] <= i[:, None], 0.0, -30000.0).astype(np.float32)
    c["strict01"] = (i[None, :] < i[:, None]).astype(np.float32)
    ct, st = rope_tables()
    c["ctab"] = ct; c["stab"] = st
    return c


def setup(nc, S, C, debug_in=None):
    C.nc = nc; C.S = S
    C.PS = [S.buf(nc.alloc_psum_tensor(f"psb{i}", [128, 512], F32).ap(), f"ps{i}") for i in range(8)]

    def psb(name, shape, dt):
        return S.buf(nc.alloc_sbuf_tensor(name, shape, dt).ap(), name)
    C.arena = None
    C.off = 0

    def sb(name, shape, dt):
        if C.arena is None:
            return psb(name, shape, dt)
        esz = 4 if dt in (F32, I32, U32) else 2
        n = int(np.prod(shape[1:]))
        words = (n * esz + 31) // 32 * 8
        assert C.off + words <= C.arena_words, (name, C.off, words, C.arena_words)
        ap = C.arena[0:shape[0], C.off:C.off + words]
        C.off += words
        if dt != F32:
            ap = ap.bitcast(dt)
        ap = ap[:, 0:n]
        if len(shape) == 3:
            ap = ap.rearrange("p (a b) -> p a b", a=shape[1])
        elif len(shape) == 4:
            ap = ap.rearrange("p (a b c) -> p a b c", a=shape[1], b=shape[2])
        return S.buf(ap, name)
    C.sb = sb

    def open_arena():
        rem = nc.sbuf_bytes_remaining() if callable(nc.sbuf_bytes_remaining) else nc.sbuf_bytes_remaining
        C.arena_words = (rem - 2048) // 4
        C.arena = nc.alloc_sbuf_tensor("arena", [128, C.arena_words], F32).ap()
    C.open_arena = open_arena

    def dram(name, shape, dt, kind="Internal"):
        return S.buf(nc.dram_tensor(name, shape, dt, kind=kind).ap(), name)
    C.dram = dram
    C.cin = {k: dram("c_" + k, list(v.shape), F32, "ExternalInput") for k, v in all_consts().items()}
    C.ident_f = sb("ident_f", [128, 128], F32)
    C.ident_b = sb("ident_b", [128, 128], BF16)
    C.ustrict_b = sb("ustrict_b", [128, 128], BF16)
    C.ones_b = sb("ones_b", [128, 128], BF16)
    C.ones_f = sb("ones_f", [128, 128], F32)
    C.ebase = sb("ebase", [128, NE], F32)
    C.tri_incl = sb("tri_incl", [128, 128], F32)
    C.negmask = sb("negmask", [128, 128], F32)
    C.strict01 = sb("strict01", [128, 128], F32)


def emit_consts(C):
    S = C.S
    S.dma("sp", C.ident_f[:], C.cin["ident"][:], reads=[C.cin["ident"]], writes=[C.ident_f])
    S.dma("pool", C.ident_b[:], C.cin["ident"][:], reads=[C.cin["ident"]], writes=[C.ident_b])
    S.dma("pool", C.ustrict_b[:], C.cin["ustrict"][:], reads=[C.cin["ustrict"]], writes=[C.ustrict_b])
    S.dma("pool", C.ones_b[:], C.cin["ones"][:], reads=[C.cin["ones"]], writes=[C.ones_b])
    S.dma("sp", C.ones_f[:], C.cin["ones"][:], reads=[C.cin["ones"]], writes=[C.ones_f])
    S.dma("sp", C.ebase[:], C.cin["ebase"][:], reads=[C.cin["ebase"]], writes=[C.ebase])
    for nm in ("tri_incl", "negmask", "strict01"):
        S.dma("sp", getattr(C, nm)[:], C.cin[nm][:], reads=[C.cin[nm]], writes=[getattr(C, nm)])


def alloc_ln(C, tag):
    L = Ctx()
    L.stats = C.sb(f"ln_stats_{tag}", [128, 2, 6], F32)
    L.mv = C.sb(f"ln_mv_{tag}", [128, 2], F32)
    L.sd = C.sb(f"ln_sd_{tag}", [128, 1], F32)
    L.rstd = C.sb(f"ln_rstd_{tag}", [128, 1], F32)
    L.nmr = C.sb(f"ln_nmr_{tag}", [128, 1], F32)
    return L


def emit_ln(C, L, t, o, wb, bb):
    S = C.S; nc = C.nc
    for h in range(2):
        S.op("dve", lambda h=h: nc.vector.bn_stats(out=L.stats[:, h, :], in_=t[:, h * 512:(h + 1) * 512]), reads=[t], writes=[L.stats])
    S.op("dve", lambda: nc.vector.bn_aggr(out=L.mv[:], in_=L.stats[:].rearrange("p a b -> p (a b)")), reads=[L.stats], writes=[L.mv])
    S.op("dve", lambda: nc.vector.tensor_scalar(out=L.sd[:], in0=L.mv[:, 1:2], scalar1=EPS, scalar2=None, op0=ALU.add), reads=[L.mv], writes=[L.sd])
    S.op("act", lambda: nc.scalar.activation(out=L.sd[:], in_=L.sd[:], func=AF.Sqrt), reads=[L.sd], writes=[L.sd])
    S.op("dve", lambda: nc.vector.reciprocal(out=L.rstd[:], in_=L.sd[:]), reads=[L.sd], writes=[L.rstd])
    S.op("dve", lambda: nc.vector.tensor_scalar(out=L.nmr[:], in0=L.mv[:, 0:1], scalar1=L.rstd[:], scalar2=-1.0, op0=ALU.mult, op1=ALU.mult), reads=[L.mv, L.rstd], writes=[L.nmr])
    S.op("act", lambda: nc.scalar.activation(out=t[:], in_=t[:], func=AF.Identity, bias=L.nmr[:], scale=L.rstd[:]), reads=[t, L.nmr, L.rstd], writes=[t])
    S.op("dve", lambda: nc.vector.tensor_tensor(out=t[:], in0=t[:], in1=wb[:], op=ALU.mult), reads=[t, wb], writes=[t])
    S.op("pool", lambda: nc.gpsimd.tensor_tensor(out=o[:], in0=t[:], in1=bb[:], op=ALU.add), reads=[t, bb], writes=[o])


def alloc_moe(C):
    M = Ctx(); sb = C.sb
    M.xt = [sb(f"moe_xt{i}", [128, D], F32) for i in range(2)]
    M.xtb = [sb(f"moe_xtb{i}", [128, D], BF16) for i in range(2)]
    M.xT = [sb(f"moe_xT{i}", [128, 8, 128], F32) for i in range(2)]
    M.wr = sb("moe_wr", [128, 8, 36], F32)
    M.br = sb("moe_br", [1, 36], F32)
    M.L = sb("moe_L", [128, NT, 36], F32)
    M.gmax = sb("moe_gmax", [128, NT], F32)
    M.goh = sb("moe_goh", [128, NT, 4], F32)
    M.gsh = sb("moe_gsh", [128, NT, 4], F32)
    M.gw = sb("moe_gw", [128, NT], F32)
    M.lem = sb("moe_lem", [128, NT, NE], F32)
    M.lem2 = sb("moe_lem2", [128, NT, NE], F32)
    M.top1 = sb("moe_top1", [128, NT], F32)
    M.top2 = sb("moe_top2", [128, NT], F32)
    M.oh1 = sb("moe_oh1", [128, NT, NE], F32)
    M.oh2 = sb("moe_oh2", [128, NT, NE], F32)
    M.ed = sb("moe_ed", [128, NT], F32)
    M.w = sb("moe_w", [128, NT, 2], F32)
    M.Mb = sb("moe_Mb", [128, NT, NE], BF16)
    M.slotmat = sb("moe_slotmat", [128, NT, NE], F32)
    M.tmp = sb("moe_tmp", [128, NT, NE], F32)
    M.slots_f = sb("moe_slots_f", [128, NT, 2], F32)
    M.slots_u = sb("moe_slots_u", [128, NT, 2], U32)
    M.wg = [sb(f"moe_wg{i}", [128, 8, FF], BF16) for i in range(2)]
    M.wu = [sb(f"moe_wu{i}", [128, 8, FF], BF16) for i in range(2)]
    M.wd = [sb(f"moe_wd{i}", [128, 4, D], BF16) for i in range(2)]
    M.xe = [sb(f"moe_xe{i}", [128, CAP // 128, D], BF16) for i in range(2)]
    M.xeT = [sb(f"moe_xeT{i}", [128, 8, CAP], BF16) for i in range(2)]
    M.sg = [sb(f"moe_sg{i}", [128, CAP], F32) for i in range(2)]
    M.hid = [sb(f"moe_hid{i}", [128, 4, CAP], BF16) for i in range(2)]
    M.ye = [sb(f"moe_ye{i}", [128, D], F32) for i in range(2)]
    M.y0 = [sb(f"moe_y0{i}", [128, D], F32) for i in range(2)]
    M.y1 = [sb(f"moe_y1{i}", [128, D], F32) for i in range(2)]
    M.acc = [sb(f"moe_acc{i}", [128, D], F32) for i in range(2)]
    M.xo = [sb(f"moe_xo{i}", [128, D], F32) for i in range(2)]
    M.lnw = sb("moe_lnw", [128, D], F32)
    M.lnb = sb("moe_lnb", [128, D], F32)
    M.ln = alloc_ln(C, "moe")
    M.xbuf = C.dram("moe_xbuf", [NE * CAP, D], BF16)
    M.ybuf = C.dram("moe_ybuf", [NE * CAP, D], F32)
    return M


def emit_moe(C, M, W, l, x1, xo):
    S = C.S; nc = C.nc; PS = C.PS
    bc = lambda ap: ap
    S.dma("sp", M.wr[:, :, 0:4], W["moe_w_group"][l].rearrange("(c p) g -> p c g", p=128), reads=[W["moe_w_group"]], writes=[M.wr])
    S.dma("sp", M.wr[:, :, 4:36], W["moe_w_expert"][l].rearrange("(c p) g -> p c g", p=128), reads=[W["moe_w_expert"]], writes=[M.wr])
    S.dma("sp", M.br[:, 0:4], W["moe_b_group"][l:l + 1, :], reads=[W["moe_b_group"]], writes=[M.br])
    S.dma("sp", M.br[:, 4:36], W["moe_b_expert"][l:l + 1, :], reads=[W["moe_b_expert"]], writes=[M.br])
    S.dma("sp", M.lnw[:], W["ln2_w"][l:l + 1, :].to_broadcast([128, D]), reads=[W["ln2_w"]], writes=[M.lnw])
    S.dma("sp", M.lnb[:], W["ln2_b"][l:l + 1, :].to_broadcast([128, D]), reads=[W["ln2_b"]], writes=[M.lnb])
    for T in range(NT):
        xt = M.xt[T % 2]; xT = M.xT[T % 2]
        S.dma("sp", xt[:], x1[T * 128:(T + 1) * 128, :], reads=[x1], writes=[xt])
        for hb in range(2):
            pb = PS[hb]
            for j in range(4):
                c = hb * 4 + j
                S.op("pe", lambda c=c, j=j, pb=pb, xt=xt: nc.tensor.transpose(out=pb[:, j * 128:(j + 1) * 128], in_=xt[:, c * 128:(c + 1) * 128], identity=C.ident_f[:]),
                     reads=[xt, C.ident_f], writes=[pb])
            eng = "act" if hb == 0 else "dve"
            if eng == "act":
                S.op("act", lambda pb=pb, xT=xT, hb=hb: nc.scalar.copy(out=xT[:, hb * 4:(hb + 1) * 4, :], in_=pb[:].rearrange("p (a b) -> p a b", a=4)), reads=[pb], writes=[xT])
            else:
                S.op("dve", lambda pb=pb, xT=xT, hb=hb: nc.vector.tensor_copy(out=xT[:, hb * 4:(hb + 1) * 4, :], in_=pb[:].rearrange("p (a b) -> p a b", a=4)), reads=[pb], writes=[xT])
        pl = PS[2 + T % 2]
        for c in range(8):
            S.op("pe", lambda c=c, pl=pl, xT=xT: nc.tensor.matmul(pl[:, 0:36], lhsT=xT[:, c, :], rhs=M.wr[:, c, :], start=(c == 0), stop=False),
                 reads=[xT, M.wr], writes=[pl])
        S.op("pe", lambda pl=pl: nc.tensor.matmul(pl[:, 0:36], lhsT=C.ones_f[0:1, :], rhs=M.br[0:1, :], start=False, stop=True), reads=[C.ones_f, M.br], writes=[pl])
        S.op("act", lambda pl=pl, T=T: nc.scalar.copy(out=M.L[:, T, :], in_=pl[:, 0:36]), reads=[pl], writes=[M.L])
    V = nc.vector
    lg = M.L[:, :, 0:4]; le = M.L[:, :, 4:36]
    S.op("dve", lambda: V.tensor_reduce(out=M.gmax[:], in_=lg, axis=AX.X, op=ALU.max), reads=[M.L], writes=[M.gmax])
    S.op("dve", lambda: V.tensor_tensor(out=M.goh[:], in0=lg, in1=M.gmax[:].unsqueeze(2).to_broadcast([128, NT, 4]), op=ALU.is_equal), reads=[M.L, M.gmax], writes=[M.goh])
    S.op("dve", lambda: V.tensor_tensor(out=M.gsh[:], in0=lg, in1=M.gmax[:].unsqueeze(2).to_broadcast([128, NT, 4]), op=ALU.subtract), reads=[M.L, M.gmax], writes=[M.gsh])
    S.op("act", lambda: nc.scalar.activation(out=M.gsh[:], in_=M.gsh[:], func=AF.Exp), reads=[M.gsh], writes=[M.gsh])
    S.op("dve", lambda: V.tensor_reduce(out=M.gw[:], in_=M.gsh[:], axis=AX.X, op=ALU.add), reads=[M.gsh], writes=[M.gw])
    S.op("dve", lambda: V.reciprocal(out=M.gw[:], in_=M.gw[:]), reads=[M.gw], writes=[M.gw])
    S.op("dve", lambda: V.tensor_scalar(out=M.goh[:], in0=M.goh[:], scalar1=1e30, scalar2=-1e30, op0=ALU.mult, op1=ALU.add), reads=[M.goh], writes=[M.goh])
    S.op("dve", lambda: V.tensor_tensor(out=M.lem[:].rearrange("p t (g e) -> p t g e", g=4), in0=le.rearrange("p t (g e) -> p t g e", g=4),
                                        in1=M.goh[:].unsqueeze(3).to_broadcast([128, NT, 4, 8]), op=ALU.add), reads=[M.L, M.goh], writes=[M.lem])
    S.op("dve", lambda: V.tensor_reduce(out=M.top1[:], in_=M.lem[:], axis=AX.X, op=ALU.max), reads=[M.lem], writes=[M.top1])
    S.op("dve", lambda: V.tensor_tensor(out=M.oh1[:], in0=M.lem[:], in1=M.top1[:].unsqueeze(2).to_broadcast([128, NT, NE]), op=ALU.is_equal), reads=[M.lem, M.top1], writes=[M.oh1])
    S.op("dve", lambda: V.scalar_tensor_tensor(out=M.lem2[:], in0=M.oh1[:], scalar=-1e30, in1=M.lem[:], op0=ALU.mult, op1=ALU.add), reads=[M.oh1, M.lem], writes=[M.lem2])
    S.op("dve", lambda: V.tensor_reduce(out=M.top2[:], in_=M.lem2[:], axis=AX.X, op=ALU.max), reads=[M.lem2], writes=[M.top2])
    S.op("dve", lambda: V.tensor_tensor(out=M.oh2[:], in0=M.lem2[:], in1=M.top2[:].unsqueeze(2).to_broadcast([128, NT, NE]), op=ALU.is_equal), reads=[M.lem2, M.top2], writes=[M.oh2])
    S.op("dve", lambda: V.tensor_tensor(out=M.ed[:], in0=M.top2[:], in1=M.top1[:], op=ALU.subtract), reads=[M.top1, M.top2], writes=[M.ed])
    S.op("act", lambda: nc.scalar.activation(out=M.ed[:], in_=M.ed[:], func=AF.Exp), reads=[M.ed], writes=[M.ed])
    S.op("dve", lambda: V.tensor_scalar(out=M.top1[:], in0=M.ed[:], scalar1=1.0, scalar2=None, op0=ALU.add), reads=[M.ed], writes=[M.top1])
    S.op("dve", lambda: V.reciprocal(out=M.top1[:], in_=M.top1[:]), reads=[M.top1], writes=[M.top1])
    S.op("dve", lambda: V.tensor_tensor(out=M.w[:, :, 0], in0=M.top1[:], in1=M.gw[:], op=ALU.mult), reads=[M.top1, M.gw], writes=[M.w])
    S.op("dve", lambda: V.tensor_tensor(out=M.w[:, :, 1], in0=M.w[:, :, 0], in1=M.ed[:], op=ALU.mult), reads=[M.w, M.ed], writes=[M.w])
    S.op("dve", lambda: V.tensor_tensor(out=M.Mb[:], in0=M.oh1[:], in1=M.oh2[:], op=ALU.add), reads=[M.oh1, M.oh2], writes=[M.Mb])
    for T in range(NT):
        pr = PS[4 + T // 16]
        col = (T % 16) * NE
        S.op("pe", lambda T=T, pr=pr, col=col: nc.tensor.matmul(pr[:, col:col + NE], lhsT=C.ustrict_b[:], rhs=M.Mb[:, T, :], start=True, stop=(T == 0)),
             reads=[C.ustrict_b, M.Mb], writes=[pr])
        for T2 in range(T):
            S.op("pe", lambda T2=T2, pr=pr, col=col, T=T: nc.tensor.matmul(pr[:, col:col + NE], lhsT=C.ones_b[:], rhs=M.Mb[:, T2, :], start=False, stop=(T2 == T - 1)),
                 reads=[C.ones_b, M.Mb], writes=[pr])
    for hb in range(2):
        S.op("dve", lambda hb=hb: V.tensor_tensor(out=M.slotmat[:, hb * 16:(hb + 1) * 16, :], in0=PS[4 + hb][:].rearrange("p (t e) -> p t e", t=16),
                                                  in1=C.ebase[:].unsqueeze(1).to_broadcast([128, 16, NE]), op=ALU.add), reads=[PS[4 + hb], C.ebase], writes=[M.slotmat])
    for k, oh in enumerate((M.oh1, M.oh2)):
        S.op("dve", lambda oh=oh: V.tensor_tensor(out=M.tmp[:], in0=M.slotmat[:], in1=oh[:], op=ALU.mult), reads=[M.slotmat, oh], writes=[M.tmp])
        S.op("dve", lambda k=k: V.tensor_reduce(out=M.slots_f[:, :, k], in_=M.tmp[:], axis=AX.X, op=ALU.add), reads=[M.tmp], writes=[M.slots_f])
    S.op("dve", lambda: V.tensor_copy(out=M.slots_u[:], in_=M.slots_f[:]), reads=[M.slots_f], writes=[M.slots_u])
    for T in range(NT):
        xt = M.xt[T % 2]; xtb = M.xtb[T % 2]
        S.dma("sp", xt[:], x1[T * 128:(T + 1) * 128, :], reads=[x1], writes=[xt])
        S.op("act", lambda xt=xt, xtb=xtb: nc.scalar.copy(out=xtb[:], in_=xt[:]), reads=[xt], writes=[xtb])
        for k in range(2):
            S.dma("pool", None, None, reads=[xtb, M.slots_u], writes=[M.xbuf],
                  fn=lambda T=T, k=k, xtb=xtb: nc.gpsimd.indirect_dma_start(out=M.xbuf[:, :], out_offset=bass.IndirectOffsetOnAxis(ap=M.slots_u[:, T, k:k + 1], axis=0),
                                                                            in_=xtb[:], in_offset=None))
    NS = CAP // 128
    for e in range(NE):
        wg = M.wg[e % 2]; wu = M.wu[e % 2]; wd = M.wd[e % 2]; xe = M.xe[e % 2]; xeT = M.xeT[e % 2]; hid = M.hid[e % 2]
        S.dma("pool", wg[:], W["moe_w_gate"][l, e].rearrange("(c p) f -> p c f", p=128), reads=[W["moe_w_gate"]], writes=[wg])
        S.dma("pool", wu[:], W["moe_w_up"][l, e].rearrange("(c p) f -> p c f", p=128), reads=[W["moe_w_up"]], writes=[wu])
        S.dma("pool", wd[:], W["moe_w_down"][l, e].rearrange("(c p) m -> p c m", p=128), reads=[W["moe_w_down"]], writes=[wd])
        S.dma("sp", xe[:], M.xbuf[e * CAP:(e + 1) * CAP, :].rearrange("(s p) d -> p s d", p=128), reads=[M.xbuf], writes=[xe])
        for s in range(NS):
            pt = PS[s % 2]
            ptb = pt[:].bitcast(BF16)
            for c in range(8):
                S.op("pe", lambda s=s, c=c, ptb=ptb, xe=xe: nc.tensor.transpose(out=ptb[:, c * 128:(c + 1) * 128], in_=xe[:, s, c * 128:(c + 1) * 128], identity=C.ident_b[:]),
                     reads=[xe, C.ident_b], writes=[pt])
            if s % 2 == 0:
                S.op("act", lambda s=s, ptb=ptb, xeT=xeT: nc.scalar.copy(out=xeT[:, :, s * 128:(s + 1) * 128], in_=ptb.rearrange("p (c t) -> p c t", c=8)), reads=[pt], writes=[xeT])
            else:
                S.op("dve", lambda s=s, ptb=ptb, xeT=xeT: V.tensor_copy(out=xeT[:, :, s * 128:(s + 1) * 128], in_=ptb.rearrange("p (c t) -> p c t", c=8)), reads=[pt], writes=[xeT])
        for fc in range(4):
            pg = PS[2 + fc % 2]; pu = PS[4 + fc % 2]; sg = M.sg[fc % 2]
            for c in range(8):
                S.op("pe", lambda c=c, fc=fc, pg=pg, wg=wg, xeT=xeT: nc.tensor.matmul(pg[:, 0:CAP], lhsT=wg[:, c, fc * 128:(fc + 1) * 128], rhs=xeT[:, c, :], start=(c == 0), stop=(c == 7)),
                     reads=[wg, xeT], writes=[pg])
            for c in range(8):
                S.op("pe", lambda c=c, fc=fc, pu=pu, wu=wu, xeT=xeT: nc.tensor.matmul(pu[:, 0:CAP], lhsT=wu[:, c, fc * 128:(fc + 1) * 128], rhs=xeT[:, c, :], start=(c == 0), stop=(c == 7)),
                     reads=[wu, xeT], writes=[pu])
            S.op("act", lambda pg=pg, sg=sg: nc.scalar.activation(out=sg[:], in_=pg[:, 0:CAP], func=AF.Silu), reads=[pg], writes=[sg])
            S.op("dve", lambda fc=fc, pu=pu, sg=sg, hid=hid: V.tensor_tensor(out=hid[:, fc, :], in0=sg[:], in1=pu[:, 0:CAP], op=ALU.mult), reads=[sg, pu], writes=[hid])
        for s in range(NS):
            ye = M.ye[s % 2]
            for mh in range(2):
                py = PS[6 + mh]
                for fc in range(4):
                    S.op("pe", lambda s=s, mh=mh, fc=fc, py=py, hid=hid, wd=wd: nc.tensor.matmul(py[:], lhsT=hid[:, fc, s * 128:(s + 1) * 128], rhs=wd[:, fc, mh * 512:(mh + 1) * 512], start=(fc == 0), stop=(fc == 3)),
                         reads=[hid, wd], writes=[py])
                if mh == 0:
                    S.op("act", lambda py=py, ye=ye: nc.scalar.copy(out=ye[:, 0:512], in_=py[:]), reads=[py], writes=[ye])
                else:
                    S.op("dve", lambda py=py, ye=ye: V.tensor_copy(out=ye[:, 512:1024], in_=py[:]), reads=[py], writes=[ye])
            r0 = e * CAP + s * 128
            S.dma("sp", M.ybuf[r0:r0 + 128, :], ye[:], reads=[ye], writes=[M.ybuf])
    for T in range(NT):
        xt = M.xt[T % 2]; y0 = M.y0[T % 2]; y1 = M.y1[T % 2]; acc = M.acc[T % 2]; o = M.xo[T % 2]
        S.dma("sp", xt[:], x1[T * 128:(T + 1) * 128, :], reads=[x1], writes=[xt])
        for k, yk in enumerate((y0, y1)):
            S.dma("pool", None, None, reads=[M.ybuf, M.slots_u], writes=[yk],
                  fn=lambda T=T, k=k, yk=yk: nc.gpsimd.indirect_dma_start(out=yk[:], out_offset=None, in_=M.ybuf[:, :],
                                                                          in_offset=bass.IndirectOffsetOnAxis(ap=M.slots_u[:, T, k:k + 1], axis=0)))
        S.op("dve", lambda T=T, y0=y0, acc=acc: V.tensor_scalar(out=acc[:], in0=y0[:], scalar1=M.w[:, T, 0:1], scalar2=None, op0=ALU.mult), reads=[y0, M.w], writes=[acc])
        S.op("dve", lambda T=T, y1=y1, acc=acc: V.scalar_tensor_tensor(out=acc[:], in0=y1[:], scalar=M.w[:, T, 1:2], in1=acc[:], op0=ALU.mult, op1=ALU.add), reads=[y1, M.w, acc], writes=[acc])
        S.op("dve", lambda xt=xt, acc=acc: V.scalar_tensor_tensor(out=acc[:], in0=xt[:], scalar=ALPHA, in1=acc[:], op0=ALU.mult, op1=ALU.add), reads=[xt, acc], writes=[acc])
        emit_ln(C, M.ln, acc, o, M.lnw, M.lnb)
        S.dma("sp", xo[T * 128:(T + 1) * 128, :], o[:], reads=[o], writes=[xo])


NCOLP = 3744
ROPE_SW = {0: 18, 1: 19, 2: 20, 3: 21, 4: 22, 6: 23, 7: 24}


def w_in_perm():
    cols = []
    hd = lambda h: list(range(h * 64, h * 64 + 64))
    for g in range(4):
        cols += hd(g) + hd(g + 4)
    kv = lambda i: list(range(512 + i * 128, 512 + (i + 1) * 128))
    cols += kv(0) + kv(1) + kv(2) + kv(4)
    cols += list(range(1304, 2072))
    cols += list(range(2336, 2848))
    sw = lambda cs: [c - (c % 64) + ((c % 64) + 8 if (c % 64) < 8 else ((c % 64) - 8 if (c % 64) < 16 else (c % 64))) for c in cs]
    for g in range(4):
        cols += sw(hd(g) + hd(g + 4))
    cols += sw(kv(0)) + sw(kv(2)) + sw(kv(4))
    cols += kv(3) + kv(5) + list(range(1280, 1304)) + list(range(2072, 2336))
    assert len(cols) == NCOLP
    return np.array(cols)


def rope_tables():
    inv = 500000.0 ** (-np.arange(0, 16, 2, dtype=np.float32) / 16)
    ang = np.arange(SEQ, dtype=np.float32)[:, None] * inv[None, :].astype(np.float32)
    cos = np.cos(ang.astype(np.float32)).astype(np.float32); sin = np.sin(ang.astype(np.float32)).astype(np.float32)
    ct = np.ones((128, SEQ), np.float32); st = np.zeros((128, SEQ), np.float32)
    for p in range(128):
        d = p % 64
        if d < 8:
            ct[p] = cos[:, d]; st[p] = -sin[:, d]
        elif d < 16:
            ct[p] = cos[:, d - 8]; st[p] = sin[:, d - 8]
    return ct, st


def alloc_scratch(C, kind="Internal"):
    Z = Ctx(); dr = lambda n, sh, dt: C.dram(n, sh, dt, kind)
    Z.QT = dr("z_QT", [4, 128, SEQ], BF16)
    Z.KcT = dr("z_KcT", [128, SEQ], BF16); Z.VcT = dr("z_VcT", [128, SEQ], BF16)
    Z.KsT = dr("z_KsT", [128, SEQ], BF16); Z.KwT = dr("z_KwT", [128, SEQ], BF16)
    Z.tok = dr("z_tok", [SEQ, 1024], F32)
    Z.gqkvT = dr("z_gqkvT", [6, 128, SEQ], F32)
    Z.uT = dr("z_uT", [2, 128, SEQ], F32)
    Z.mixT = dr("z_mixT", [8, 128, SEQ], BF16)
    Z.x1 = dr("z_x1", [SEQ, D], F32)
    Z.xa = dr("z_xa", [SEQ, D], F32)
    Z.xb = dr("z_xb", [SEQ, D], F32)
    return Z


def alloc_proj(C):
    A = Ctx(); sb = C.sb
    A.w = sb("pj_w", [128, 8, NCOLP], BF16)
    A.xT = sb("pj_xT", [128, 8, SEQ], BF16)
    A.xt = [sb(f"pj_xt{i}", [128, D], F32) for i in range(2)]
    A.ct = [sb(f"pj_ct{i}", [128, 512], F32) for i in range(2)]
    A.st = [sb(f"pj_st{i}", [128, 512], F32) for i in range(2)]
    A.t1 = [sb(f"pj_t1{i}", [128, 512], F32) for i in range(2)]
    A.t2 = [sb(f"pj_t2{i}", [128, 512], F32) for i in range(2)]
    A.ob = [sb(f"pj_ob{i}", [128, 512], BF16) for i in range(2)]
    A.of = [sb(f"pj_of{i}", [128, 512], F32) for i in range(2)]
    A.zb = [sb(f"pj_zb{i}", [128, 1024], F32) for i in range(2)]
    return A


def emit_proj(C, A, Z, W, l, x):
    S = C.S; nc = C.nc; PS = C.PS; V = nc.vector
    k = 0
    for c in range(8):
        for h0 in range(0, NCOLP, 1024):
            h1 = min(NCOLP, h0 + 1024)
            stg = A.xt[k % 2]
            S.dma("sp", stg[:, 0:h1 - h0], W["w_in_p"][l, c * 128:(c + 1) * 128, h0:h1], reads=[W["w_in_p"]], writes=[stg])
            if k % 2 == 0:
                S.op("act", lambda stg=stg, c=c, h0=h0, h1=h1: nc.scalar.copy(out=A.w[:, c, h0:h1], in_=stg[:, 0:h1 - h0]), reads=[stg], writes=[A.w])
            else:
                S.op("dve", lambda stg=stg, c=c, h0=h0, h1=h1: V.tensor_copy(out=A.w[:, c, h0:h1], in_=stg[:, 0:h1 - h0]), reads=[stg], writes=[A.w])
            k += 1
    for T in range(NT):
        xt = A.xt[T % 2]
        S.dma("sp", xt[:], x[T * 128:(T + 1) * 128, :], reads=[x], writes=[xt])
        for hb in range(2):
            pb = PS[(T % 2) * 2 + hb]
            for j in range(4):
                c = hb * 4 + j
                S.op("pe", lambda c=c, j=j, pb=pb, xt=xt: nc.tensor.transpose(out=pb[:, j * 128:(j + 1) * 128], in_=xt[:, c * 128:(c + 1) * 128], identity=C.ident_f[:]),
                     reads=[xt, C.ident_f], writes=[pb])
            dst = A.xT[:, hb * 4:(hb + 1) * 4, T * 128:(T + 1) * 128]
            if hb == 0:
                S.op("act", lambda pb=pb, dst=dst: nc.scalar.copy(out=dst, in_=pb[:].rearrange("p (a b) -> p a b", a=4)), reads=[pb], writes=[A.xT])
            else:
                S.op("dve", lambda pb=pb, dst=dst: V.tensor_copy(out=dst, in_=pb[:].rearrange("p (a b) -> p a b", a=4)), reads=[pb], writes=[A.xT])
    import os as _os
    _ph = int(_os.environ.get("PROJ_PH", "3"))
    if _ph < 2:
        return
    cnt = [0]

    def blockmm(ps, blk, tg):
        for c in range(8):
            S.op("pe", lambda c=c: nc.tensor.matmul(ps[:], lhsT=A.w[:, c, blk * 128:(blk + 1) * 128], rhs=A.xT[:, c, tg * 512:(tg + 1) * 512], start=(c == 0), stop=(c == 7)),
                 reads=[A.w, A.xT], writes=[ps])
    dests = {0: lambda sl: Z.QT[0][:, sl], 1: lambda sl: Z.QT[1][:, sl], 2: lambda sl: Z.QT[2][:, sl], 3: lambda sl: Z.QT[3][:, sl],
             4: lambda sl: Z.KcT[:, sl], 5: lambda sl: Z.VcT[:, sl], 6: lambda sl: Z.KsT[:, sl], 7: lambda sl: Z.KwT[:, sl]}
    dbuf = {0: Z.QT, 1: Z.QT, 2: Z.QT, 3: Z.QT, 4: Z.KcT, 5: Z.VcT, 6: Z.KsT, 7: Z.KwT}
    for tg in range(8):
        sl = slice(tg * 512, (tg + 1) * 512)
        ct = A.ct[tg % 2]; st = A.st[tg % 2]
        S.dma("sp", ct[:], C.cin["ctab"][:, sl], reads=[C.cin["ctab"]], writes=[ct])
        S.dma("sp", st[:], C.cin["stab"][:, sl], reads=[C.cin["stab"]], writes=[st])
        for blk in range(14):
            k = cnt[0]; cnt[0] += 1
            ps = PS[(k % 2) * 2]; ps2 = PS[(k % 2) * 2 + 1]
            blockmm(ps, blk, tg)
            if blk in ROPE_SW:
                blockmm(ps2, ROPE_SW[blk], tg)
                t1 = A.t1[k % 2]; t2 = A.t2[k % 2]; ob = A.ob[k % 2]
                S.op("dve", lambda ps=ps, t1=t1, ct=ct: V.tensor_tensor(out=t1[:], in0=ps[:], in1=ct[:], op=ALU.mult), reads=[ps, ct], writes=[t1])
                S.op("dve", lambda ps2=ps2, t2=t2, st=st: V.tensor_tensor(out=t2[:], in0=ps2[:], in1=st[:], op=ALU.mult), reads=[ps2, st], writes=[t2])
                S.op("dve", lambda t1=t1, t2=t2, ob=ob: V.tensor_tensor(out=ob[:], in0=t1[:], in1=t2[:], op=ALU.add), reads=[t1, t2], writes=[ob])
                S.dma("sp", dests[blk](sl), ob[:], reads=[ob], writes=[dbuf[blk]])
            elif blk == 5:
                ob = A.ob[k % 2]
                S.op("act", lambda ps=ps, ob=ob: nc.scalar.copy(out=ob[:], in_=ps[:]), reads=[ps], writes=[ob])
                S.dma("sp", dests[blk](sl), ob[:], reads=[ob], writes=[dbuf[blk]])
            else:
                of = A.of[k % 2]
                S.op("act", lambda ps=ps, of=of: nc.scalar.copy(out=of[:], in_=ps[:]), reads=[ps], writes=[of])
                S.dma("sp", Z.gqkvT[blk - 8][:, sl], of[:], reads=[of], writes=[Z.gqkvT])
        for j in range(2):
            k = cnt[0]; cnt[0] += 1
            ps = PS[4 + (k % 2) * 2]; ps2 = PS[4 + (k % 2) * 2 + 1]
            blockmm(ps, 14 + j, tg)
            blockmm(ps2, 16 + j, tg)
            t1 = A.t1[k % 2]; of = A.of[k % 2]
            S.op("act", lambda ps2=ps2, t1=t1: nc.scalar.activation(out=t1[:], in_=ps2[:], func=AF.Sigmoid), reads=[ps2], writes=[t1])
            S.op("dve", lambda ps=ps, t1=t1, of=of: V.tensor_tensor(out=of[:], in0=ps[:], in1=t1[:], op=ALU.mult), reads=[ps, t1], writes=[of])
            S.dma("sp", Z.uT[j][:, sl], of[:], reads=[of], writes=[Z.uT])
    if _ph < 3:
        return
    for T in range(NT):
        pa = PS[(T % 2) * 2]; pz = PS[(T % 2) * 2 + 1]
        for c in range(8):
            S.op("pe", lambda c=c, pa=pa, T=T: nc.tensor.matmul(pa[:, 0:280], lhsT=A.xT[:, c, T * 128:(T + 1) * 128], rhs=A.w[:, c, 3200:3480], start=(c == 0), stop=(c == 7)),
                 reads=[A.w, A.xT], writes=[pa])
        for c in range(8):
            S.op("pe", lambda c=c, pz=pz, T=T: nc.tensor.matmul(pz[:, 0:264], lhsT=A.xT[:, c, T * 128:(T + 1) * 128], rhs=A.w[:, c, 3480:3744], start=(c == 0), stop=(c == 7)),
                 reads=[A.w, A.xT], writes=[pz])
        zb = A.zb[T % 2]
        S.op("dve", lambda pa=pa, zb=zb: V.tensor_copy(out=zb[:, 512:640].bitcast(BF16), in_=pa[:, 0:256]), reads=[pa], writes=[zb])
        S.op("act", lambda pa=pa, zb=zb: nc.scalar.activation(out=zb[:, 264:288], in_=pa[:, 256:280], func=AF.Sigmoid), reads=[pa, zb], writes=[zb])
        S.op("act", lambda pz=pz, zb=zb: nc.scalar.copy(out=zb[:, 0:264], in_=pz[:, 0:264]), reads=[pz, zb], writes=[zb])
        if T < 2:
            S.op("dve", lambda zb=zb: V.memset(zb[:, 288:512], 0.0), reads=[zb], writes=[zb])
            S.op("dve", lambda zb=zb: V.memset(zb[:, 640:1024], 0.0), reads=[zb], writes=[zb])
        tsl = slice(T * 128, (T + 1) * 128)
        S.dma("sp", Z.tok[tsl, :], zb[:], reads=[zb], writes=[Z.tok])


def alloc_conf(C):
    F = Ctx(); sb = C.sb
    F.u = [sb(f"cf_u{j}", [128, SEQ + 32], F32) for j in range(2)]
    F.acc = [sb(f"cf_acc{j}", [128, SEQ], F32) for j in range(2)]
    F.sq = [sb(f"cf_sq{j}", [128, 512], F32) for j in range(2)]
    F.raw = sb("cf_raw", [34, 256], F32)
    F.dwp = sb("cf_dwp", [128, 2, 34], F32)
    F.mean = sb("cf_mean", [128, 512], F32)
    F.var = sb("cf_var", [128, 512], F32)
    F.rstd = sb("cf_rstd", [128, 512], F32)
    F.t = [sb(f"cf_t{j}", [128, 512], F32) for j in range(2)]
    F.ob = [sb(f"cf_ob{j}", [128, 512], BF16) for j in range(2)]
    F.avg = sb("cf_avg", [128, 128], F32)
    return F


def emit_conf(C, F, Z, W, l):
    S = C.S; nc = C.nc; PS = C.PS; V = nc.vector
    S.dma("sp", F.raw[0:31, :], W["conf_dw_w"][l], reads=[W["conf_dw_w"]], writes=[F.raw])
    for i, nm in enumerate(("conf_dw_b", "conf_ln_w", "conf_ln_b")):
        S.dma("sp", F.raw[31 + i:32 + i, :], W[nm][l:l + 1, :], reads=[W[nm]], writes=[F.raw])
    for j in range(2):
        S.op("pe", lambda j=j: nc.tensor.transpose(out=PS[7][:, j * 34:(j + 1) * 34], in_=F.raw[0:34, j * 128:(j + 1) * 128], identity=C.ident_f[0:34, 0:34]), reads=[F.raw, C.ident_f], writes=[PS[7]])
    S.op("act", lambda: nc.scalar.copy(out=F.dwp[:], in_=PS[7][:, 0:68].rearrange("p (j k) -> p j k", j=2)), reads=[PS[7]], writes=[F.dwp])
    S.op("dve", lambda: V.tensor_scalar(out=F.avg[:], in0=C.ones_f[:], scalar1=1.0 / 256, scalar2=None, op0=ALU.mult), reads=[C.ones_f], writes=[F.avg])
    for j in range(2):
        u = F.u[j]; acc = F.acc[j]
        S.op("pool", lambda u=u: nc.gpsimd.memset(u[:, 0:32], 0.0), writes=[u])
        S.dma("sp", u[:, 32:32 + SEQ], Z.uT[j], reads=[Z.uT], writes=[u])
        S.op("dve", lambda u=u, acc=acc, j=j: V.tensor_scalar(out=acc[:], in0=u[:, 32:32 + SEQ], scalar1=F.dwp[:, j, 30:31], scalar2=F.dwp[:, j, 31:32], op0=ALU.mult, op1=ALU.add),
             reads=[u, F.dwp], writes=[acc])
        for k in range(30):
            off = 32 - 30 + k
            S.op("dve", lambda u=u, acc=acc, j=j, k=k, off=off: V.scalar_tensor_tensor(out=acc[:], in0=u[:, off:off + SEQ], scalar=F.dwp[:, j, k:k + 1], in1=acc[:], op0=ALU.mult, op1=ALU.add),
                 reads=[u, F.dwp, acc], writes=[acc])
    for tg in range(8):
        sl = slice(tg * 512, (tg + 1) * 512)
        pm = PS[(tg % 2) * 2]; pq = PS[(tg % 2) * 2 + 1]
        for j in range(2):
            S.op("act", lambda j=j: nc.scalar.activation(out=F.sq[j][:], in_=F.acc[j][:, sl], func=AF.Square), reads=[F.acc[j]], writes=[F.sq[j]])
        for j in range(2):
            S.op("pe", lambda j=j: nc.tensor.matmul(pm[:], lhsT=F.avg[:], rhs=F.acc[j][:, sl], start=(j == 0), stop=(j == 1)), reads=[F.avg, F.acc[j]], writes=[pm])
        for j in range(2):
            S.op("pe", lambda j=j: nc.tensor.matmul(pq[:], lhsT=F.avg[:], rhs=F.sq[j][:], start=(j == 0), stop=(j == 1)), reads=[F.avg, F.sq[j]], writes=[pq])
        S.op("act", lambda: nc.scalar.copy(out=F.mean[:], in_=pm[:]), reads=[pm], writes=[F.mean])
        S.op("dve", lambda: V.tensor_tensor(out=F.var[:], in0=F.mean[:], in1=F.mean[:], op=ALU.mult), reads=[F.mean], writes=[F.var])
        S.op("dve", lambda: V.tensor_tensor(out=F.var[:], in0=pq[:], in1=F.var[:], op=ALU.subtract), reads=[pq, F.var], writes=[F.var])
        S.op("dve", lambda: V.tensor_scalar(out=F.var[:], in0=F.var[:], scalar1=EPS, scalar2=None, op0=ALU.add), reads=[F.var], writes=[F.var])
        S.op("act", lambda: nc.scalar.activation(out=F.var[:], in_=F.var[:], func=AF.Sqrt), reads=[F.var], writes=[F.var])
        S.op("dve", lambda: V.reciprocal(out=F.rstd[:], in_=F.var[:]), reads=[F.var], writes=[F.rstd])
        for j in range(2):
            t = F.t[j]; ob = F.ob[j]
            S.op("dve", lambda j=j, t=t: V.tensor_tensor(out=t[:], in0=F.acc[j][:, sl], in1=F.mean[:], op=ALU.subtract), reads=[F.acc[j], F.mean], writes=[t])
            S.op("dve", lambda t=t: V.tensor_tensor(out=t[:], in0=t[:], in1=F.rstd[:], op=ALU.mult), reads=[t, F.rstd], writes=[t])
            S.op("act", lambda j=j, t=t, ob=ob: nc.scalar.activation(out=ob[:], in_=t[:], func=AF.Silu, bias=F.dwp[:, j, 33:34], scale=F.dwp[:, j, 32:33]), reads=[t, F.dwp], writes=[ob])
            S.dma("sp", Z.mixT[6 + j][:, sl], ob[:], reads=[ob], writes=[Z.mixT])


def alloc_outp(C):
    O = Ctx(); sb = C.sb
    O.w = sb("op_w", [128, 8, D], BF16)
    O.mT = [sb(f"op_mT{i}", [128, 8, 128], BF16) for i in range(2)]
    O.xt = [sb(f"op_xt{i}", [128, D], F32) for i in range(2)]
    O.acc = [sb(f"op_acc{i}", [128, D], F32) for i in range(2)]
    O.o = [sb(f"op_o{i}", [128, D], F32) for i in range(2)]
    O.lnw = sb("op_lnw", [128, D], F32)
    O.lnb = sb("op_lnb", [128, D], F32)
    O.ln = alloc_ln(C, "op")
    return O


def emit_outp(C, O, Z, W, l, x, x1):
    S = C.S; nc = C.nc; PS = C.PS; V = nc.vector
    S.dma("pool", O.w[:], W["w_out"][l].rearrange("(c p) m -> p c m", p=128), reads=[W["w_out"]], writes=[O.w])
    S.dma("sp", O.lnw[:], W["ln1_w"][l:l + 1, :].to_broadcast([128, D]), reads=[W["ln1_w"]], writes=[O.lnw])
    S.dma("sp", O.lnb[:], W["ln1_b"][l:l + 1, :].to_broadcast([128, D]), reads=[W["ln1_b"]], writes=[O.lnb])
    for T in range(NT):
        mT = O.mT[T % 2]; xt = O.xt[T % 2]; acc = O.acc[T % 2]; o = O.o[T % 2]
        tsl = slice(T * 128, (T + 1) * 128)
        S.dma("sp", mT[:], Z.mixT[:, :, tsl].rearrange("c p t -> p c t"), reads=[Z.mixT], writes=[mT])
        S.dma("sp", xt[:], x[tsl, :], reads=[x], writes=[xt])
        for mh in range(2):
            ps = PS[(T % 2) * 2 + mh]
            for c in range(8):
                S.op("pe", lambda c=c, ps=ps, mT=mT, mh=mh: nc.tensor.matmul(ps[:], lhsT=mT[:, c, :], rhs=O.w[:, c, mh * 512:(mh + 1) * 512], start=(c == 0), stop=(c == 7)),
                     reads=[mT, O.w], writes=[ps])
            S.op("dve", lambda ps=ps, xt=xt, acc=acc, mh=mh: V.scalar_tensor_tensor(out=acc[:, mh * 512:(mh + 1) * 512], in0=xt[:, mh * 512:(mh + 1) * 512], scalar=ALPHA, in1=ps[:], op0=ALU.mult, op1=ALU.add),
                 reads=[ps, xt], writes=[acc])
        emit_ln(C, O.ln, acc, o, O.lnw, O.lnb)
        S.dma("sp", x1[tsl, :], o[:], reads=[o], writes=[x1])


GC = 8
NHG = GC * 4


def alloc_gdn(C, Z):
    Gd = Ctx(); sb = C.sb
    Gd.hT = C.dram("z_ghT", [6, 128, SEQ], F32)
    Gd.craw = sb("g_craw", [4, 768], F32)
    Gd.cw = sb("g_cw", [128, 6, 4], F32)
    Gd.hd = sb("g_hd", [64, 3, 4], F32)
    Gd.nw = sb("g_nw", [64, 64], F32)
    Gd.S = sb("g_S", [64, 4, 64], F32)
    off0 = C.off
    Gd.xin = sb("g_xin", [128, SEQ + 4], F32)
    Gd.acc = sb("g_acc", [128, SEQ], F32)
    C.off = off0
    Gd.hTg = sb("g_hTg", [128, 6, 512], F32)
    Gd.zba = sb("g_zba", [64, GC, 264], F32)
    Gd.H = sb("g_H", [64, GC, 768], F32)
    Gd.sq = sb("g_sq", [64, GC, 512], F32)
    Gd.ss = sb("g_ss", [64, GC, 8], F32)
    Gd.beta = sb("g_beta", [64, NHG], F32)
    Gd.nbeta = sb("g_nbeta", [64, NHG], F32)
    Gd.g = sb("g_g", [64, NHG], F32)
    Gd.gcum = sb("g_gcum", [64, NHG], F32)
    Gd.eg = sb("g_eg", [64, NHG], F32)
    Gd.egl = sb("g_egl", [64, NHG], F32)
    Gd.ekd = sb("g_ekd", [64, NHG], F32)
    Gd.bg = sb("g_bg", [64, NHG], F32)
    Gd.U = sb("g_U", [64, NHG, 128], F32)
    Gd.kdec = sb("g_kdec", [64, NHG, 64], F32)
    Gd.qdec = sb("g_qdec", [64, NHG, 64], F32)
    Gd.DG = sb("g_DG", [64, NHG, 64], F32)
    Gd.nDG = sb("g_nDG", [64, NHG, 64], F32)
    Gd.kT = sb("g_kT", [64, 8, 64], F32)
    Gd.qT = sb("g_qT", [64, 8, 64], F32)
    Gd.dec = sb("g_dec", [64, 8, 64], F32)
    Gd.B = [sb(f"g_B{i}", [64, 8, 64], F32) for i in range(2)]
    Gd.BT = [sb(f"g_BT{i}", [64, 8, 64], F32) for i in range(2)]
    Gd.qk = sb("g_qk", [64, 8, 64], F32)
    Gd.wT = sb("g_wT", [64, NHG, 64], F32)
    Gd.qdT = sb("g_qdT", [64, NHG, 64], F32)
    Gd.qkT = sb("g_qkT", [64, NHG, 64], F32)
    Gd.vn = sb("g_vn", [64, 4, 64], F32)
    Gd.o = sb("g_o", [64, GC, 256], F32)
    Gd.ms = sb("g_ms", [64, GC, 4], F32)
    Gd.sz = sb("g_sz", [64, GC, 256], F32)
    Gd.y = sb("g_y", [64, GC, 256], BF16)
    Gd.ymT = sb("g_ymT", [128, 2, 512], BF16)
    return Gd


def emit_gdn(C, Gd, Z, W, l):
    S = C.S; nc = C.nc; PS = C.PS; V = nc.vector; A_ = nc.scalar
    I64 = C.ident_f[0:64, 0:64]; O64 = C.ones_f[0:64, 0:64]
    S.dma("sp", Gd.craw[:], W["gdn_conv_w"][l], reads=[W["gdn_conv_w"]], writes=[Gd.craw])
    for b in range(6):
        S.op("pe", lambda b=b: nc.tensor.transpose(out=PS[7][:, b * 4:(b + 1) * 4], in_=Gd.craw[0:4, b * 128:(b + 1) * 128], identity=C.ident_f[0:4, 0:4]), reads=[Gd.craw, C.ident_f], writes=[PS[7]])
    S.op("act", lambda: A_.copy(out=Gd.cw[:], in_=PS[7][:, 0:24].rearrange("p (b k) -> p b k", b=6)), reads=[PS[7]], writes=[Gd.cw])
    S.dma("sp", Gd.hd[:, 0, :], W["gdn_a_log"][l:l + 1, :].to_broadcast([64, 4]), reads=[W["gdn_a_log"]], writes=[Gd.hd])
    S.dma("sp", Gd.hd[:, 1, :], W["gdn_dt_bias"][l:l + 1, :].to_broadcast([64, 4]), reads=[W["gdn_dt_bias"]], writes=[Gd.hd])
    S.dma("sp", Gd.nw[:], W["gdn_norm_w"][l:l + 1, :].to_broadcast([64, 64]), reads=[W["gdn_norm_w"]], writes=[Gd.nw])
    S.op("act", lambda: A_.activation(out=Gd.hd[:, 2, :], in_=Gd.hd[:, 0, :], func=AF.Exp), reads=[Gd.hd], writes=[Gd.hd])
    S.op("dve", lambda: V.tensor_scalar(out=Gd.hd[:, 2, :], in0=Gd.hd[:, 2, :], scalar1=-1.0, scalar2=None, op0=ALU.mult), reads=[Gd.hd], writes=[Gd.hd])
    S.op("dve", lambda: V.memset(Gd.S[:], 0.0), writes=[Gd.S])
    S.op("pool", lambda: nc.gpsimd.memset(Gd.xin[:, 0:4], 0.0), writes=[Gd.xin])
    for b in range(6):
        S.dma("sp", Gd.xin[:, 4:4 + SEQ], Z.gqkvT[b], reads=[Z.gqkvT], writes=[Gd.xin])
        S.op("dve", lambda b=b: V.tensor_scalar(out=Gd.acc[:], in0=Gd.xin[:, 4:4 + SEQ], scalar1=Gd.cw[:, b, 3:4], scalar2=None, op0=ALU.mult), reads=[Gd.xin, Gd.cw], writes=[Gd.acc])
        for k in range(3):
            S.op("dve", lambda b=b, k=k: V.scalar_tensor_tensor(out=Gd.acc[:], in0=Gd.xin[:, 1 + k:1 + k + SEQ], scalar=Gd.cw[:, b, k:k + 1], in1=Gd.acc[:], op0=ALU.mult, op1=ALU.add),
                 reads=[Gd.xin, Gd.cw, Gd.acc], writes=[Gd.acc])
        S.op("act", lambda: A_.activation(out=Gd.acc[:], in_=Gd.acc[:], func=AF.Silu), reads=[Gd.acc], writes=[Gd.acc])
        S.dma("sp", Gd.hT[b], Gd.acc[:], reads=[Gd.acc], writes=[Gd.hT])
    S.barrier()
    b3 = lambda ap, n: ap.unsqueeze(2).to_broadcast([64, NHG, n])
    for cg in range(SEQ // (64 * GC)):
        t0 = cg * 64 * GC
        S.dma("sp", Gd.hTg[:], Gd.hT[:, :, t0:t0 + 512].rearrange("b p t -> p b t"), reads=[Gd.hT], writes=[Gd.hTg])
        S.dma("sp", Gd.zba[:], Z.tok[t0:t0 + 512, 0:264].rearrange("(n i) c -> i n c", i=64), reads=[Z.tok], writes=[Gd.zba])
        for n in range(GC):
            pa = PS[(n % 2) * 2]; pb = PS[(n % 2) * 2 + 1]
            for b in range(6):
                pp = pa if b < 4 else pb
                S.op("pe", lambda n=n, b=b, pp=pp: nc.tensor.transpose(out=pp[0:64, (b % 4) * 128:(b % 4 + 1) * 128], in_=Gd.hTg[:, b, n * 64:(n + 1) * 64], identity=C.ident_f[:]),
                     reads=[Gd.hTg, C.ident_f], writes=[pp])
            S.op("act", lambda n=n, pa=pa: A_.copy(out=Gd.H[:, n, 0:512], in_=pa[0:64, :]), reads=[pa], writes=[Gd.H])
            S.op("dve", lambda n=n, pb=pb: V.tensor_copy(out=Gd.H[:, n, 512:768], in_=pb[0:64, 0:256]), reads=[pb], writes=[Gd.H])
        S.op("act", lambda: A_.activation(out=Gd.sq[:], in_=Gd.H[:, :, 0:512], func=AF.Square), reads=[Gd.H], writes=[Gd.sq])
        S.op("dve", lambda: V.tensor_reduce(out=Gd.ss[:], in_=Gd.sq[:].rearrange("p n (h d) -> p n h d", h=8), axis=AX.X, op=ALU.add), reads=[Gd.sq], writes=[Gd.ss])
        S.op("dve", lambda: V.tensor_scalar(out=Gd.ss[:], in0=Gd.ss[:], scalar1=1e-6, scalar2=None, op0=ALU.add), reads=[Gd.ss], writes=[Gd.ss])
        S.op("act", lambda: A_.activation(out=Gd.ss[:], in_=Gd.ss[:], func=AF.Sqrt), reads=[Gd.ss], writes=[Gd.ss])
        S.op("dve", lambda: V.reciprocal(out=Gd.ss[:], in_=Gd.ss[:]), reads=[Gd.ss], writes=[Gd.ss])
        S.op("dve", lambda: V.tensor_scalar(out=Gd.ss[:, :, 0:4], in0=Gd.ss[:, :, 0:4], scalar1=0.125, scalar2=None, op0=ALU.mult), reads=[Gd.ss], writes=[Gd.ss])
        S.op("dve", lambda: V.tensor_tensor(out=Gd.H[:, :, 0:512].rearrange("p n (h d) -> p n h d", h=8), in0=Gd.H[:, :, 0:512].rearrange("p n (h d) -> p n h d", h=8),
                                            in1=Gd.ss[:].unsqueeze(3).to_broadcast([64, GC, 8, 64]), op=ALU.mult), reads=[Gd.H, Gd.ss], writes=[Gd.H])
        v4 = lambda ap: ap.rearrange("p (n h) -> p n h", h=4)
        S.op("act", lambda: A_.activation(out=v4(Gd.beta[:]), in_=Gd.zba[:, :, 256:260], func=AF.Sigmoid), reads=[Gd.zba], writes=[Gd.beta])
        S.op("dve", lambda: V.tensor_scalar(out=Gd.nbeta[:], in0=Gd.beta[:], scalar1=-1.0, scalar2=None, op0=ALU.mult), reads=[Gd.beta], writes=[Gd.nbeta])
        S.op("dve", lambda: V.tensor_tensor(out=v4(Gd.g[:]), in0=Gd.zba[:, :, 260:264], in1=Gd.hd[:, 1, :].unsqueeze(1).to_broadcast([64, GC, 4]), op=ALU.add), reads=[Gd.zba, Gd.hd], writes=[Gd.g])
        S.op("act", lambda: A_.activation(out=Gd.g[:], in_=Gd.g[:], func=AF.Exp), reads=[Gd.g], writes=[Gd.g])
        S.op("act", lambda: A_.activation(out=Gd.g[:], in_=Gd.g[:], func=AF.Ln, bias=1.0), reads=[Gd.g], writes=[Gd.g])
        S.op("dve", lambda: V.tensor_tensor(out=v4(Gd.g[:]), in0=v4(Gd.g[:]), in1=Gd.hd[:, 2, :].unsqueeze(1).to_broadcast([64, GC, 4]), op=ALU.mult), reads=[Gd.g, Gd.hd], writes=[Gd.g])
        pg = PS[4]
        S.op("pe", lambda: nc.tensor.matmul(pg[0:64, 0:NHG], lhsT=C.tri_incl[0:64, 0:64], rhs=Gd.g[:], start=True, stop=True), reads=[C.tri_incl, Gd.g], writes=[pg])
        S.op("pe", lambda: nc.tensor.matmul(pg[0:64, NHG:2 * NHG], lhsT=O64, rhs=Gd.g[:], start=True, stop=True), reads=[C.ones_f, Gd.g], writes=[pg])
        S.op("dve", lambda: V.tensor_copy(out=Gd.gcum[:], in_=pg[0:64, 0:NHG]), reads=[pg], writes=[Gd.gcum])
        S.op("act", lambda: A_.activation(out=Gd.eg[:], in_=pg[0:64, 0:NHG], func=AF.Exp), reads=[pg], writes=[Gd.eg])
        S.op("act", lambda: A_.activation(out=Gd.egl[:], in_=pg[0:64, NHG:2 * NHG], func=AF.Exp), reads=[pg], writes=[Gd.egl])
        S.op("dve", lambda: V.tensor_tensor(out=Gd.ekd[:], in0=pg[0:64, NHG:2 * NHG], in1=Gd.gcum[:], op=ALU.subtract), reads=[pg, Gd.gcum], writes=[Gd.ekd])
        S.op("act", lambda: A_.activation(out=Gd.ekd[:], in_=Gd.ekd[:], func=AF.Exp), reads=[Gd.ekd], writes=[Gd.ekd])
        S.op("dve", lambda: V.tensor_tensor(out=Gd.bg[:], in0=Gd.beta[:], in1=Gd.eg[:], op=ALU.mult), reads=[Gd.beta, Gd.eg], writes=[Gd.bg])
        hv = lambda lo: Gd.H[:, :, lo:lo + 256].rearrange("p n (h d) -> p n h d", h=4)
        u4 = lambda ap, lo: ap[:, :, lo:lo + 64].rearrange("p (n h) d -> p n h d", h=4)
        bb = lambda ap: ap.rearrange("p (n h) -> p n h", h=4).unsqueeze(3).to_broadcast([64, GC, 4, 64])
        S.op("dve", lambda: V.tensor_tensor(out=u4(Gd.U[:], 0), in0=hv(512), in1=bb(Gd.beta[:]), op=ALU.mult), reads=[Gd.H, Gd.beta], writes=[Gd.U])
        S.op("dve", lambda: V.tensor_tensor(out=u4(Gd.U[:], 64), in0=hv(256), in1=bb(Gd.bg[:]), op=ALU.mult), reads=[Gd.H, Gd.bg, Gd.U], writes=[Gd.U])
        S.op("dve", lambda: V.tensor_tensor(out=u4(Gd.kdec[:], 0), in0=hv(256), in1=bb(Gd.ekd[:]), op=ALU.mult), reads=[Gd.H, Gd.ekd], writes=[Gd.kdec])
        S.op("dve", lambda: V.tensor_tensor(out=u4(Gd.qdec[:], 0), in0=hv(0), in1=bb(Gd.eg[:]), op=ALU.mult), reads=[Gd.H, Gd.eg], writes=[Gd.qdec])
        S.op("dve", lambda: V.tensor_tensor(out=Gd.DG[:], in0=I64.unsqueeze(1).to_broadcast([64, NHG, 64]), in1=b3(Gd.gcum[:], 64), op=ALU.mult), reads=[C.ident_f, Gd.gcum], writes=[Gd.DG])
        S.op("dve", lambda: V.tensor_scalar(out=Gd.nDG[:], in0=Gd.DG[:], scalar1=-1.0, scalar2=None, op0=ALU.mult), reads=[Gd.DG], writes=[Gd.nDG])
        for sbi in range(NHG // 8):
            base = sbi * 8
            hsel = lambda lo, m: Gd.H[:, (base + m) // 4, lo + ((base + m) % 4) * 64: lo + ((base + m) % 4) * 64 + 64]
            pk, pq, pqd, pD, pKK, pQK = PS[0], PS[1], PS[2], PS[3], PS[5], PS[6]
            for m in range(8):
                S.op("pe", lambda m=m: nc.tensor.transpose(out=pk[0:64, m * 64:(m + 1) * 64], in_=hsel(256, m), identity=I64), reads=[Gd.H, C.ident_f], writes=[pk])
                S.op("pe", lambda m=m: nc.tensor.transpose(out=pq[0:64, m * 64:(m + 1) * 64], in_=hsel(0, m), identity=I64), reads=[Gd.H, C.ident_f], writes=[pq])
                S.op("pe", lambda m=m: nc.tensor.transpose(out=pqd[0:64, m * 64:(m + 1) * 64], in_=Gd.qdec[:, base + m, :], identity=I64), reads=[Gd.qdec, C.ident_f], writes=[pqd])
            f8 = lambda ap: ap.rearrange("p m d -> p (m d)")
            S.op("act", lambda: A_.copy(out=f8(Gd.kT[:]), in_=pk[0:64, :]), reads=[pk], writes=[Gd.kT])
            S.op("dve", lambda: V.tensor_copy(out=f8(Gd.qT[:]), in_=pq[0:64, :]), reads=[pq], writes=[Gd.qT])
            S.op("act", lambda: A_.copy(out=f8(Gd.qdT[:, base:base + 8, :]), in_=pqd[0:64, :]), reads=[pqd], writes=[Gd.qdT])
            for m in range(8):
                o_ = pD[0:64, m * 64:(m + 1) * 64]
                S.op("pe", lambda m=m, o_=o_: nc.tensor.matmul(o_, lhsT=Gd.DG[:, base + m, :], rhs=O64, start=True, stop=False), reads=[Gd.DG, C.ones_f], writes=[pD])
                S.op("pe", lambda m=m, o_=o_: nc.tensor.matmul(o_, lhsT=O64, rhs=Gd.nDG[:, base + m, :], start=False, stop=False), reads=[Gd.nDG, C.ones_f], writes=[pD])
                S.op("pe", lambda m=m, o_=o_: nc.tensor.matmul(o_, lhsT=I64, rhs=C.negmask[0:64, 0:64], start=False, stop=True), reads=[C.negmask, C.ident_f], writes=[pD])
                S.op("pe", lambda m=m: nc.tensor.matmul(pKK[0:64, m * 64:(m + 1) * 64], lhsT=Gd.kT[:, m, :], rhs=Gd.kT[:, m, :], start=True, stop=True), reads=[Gd.kT], writes=[pKK])
                S.op("pe", lambda m=m: nc.tensor.matmul(pQK[0:64, m * 64:(m + 1) * 64], lhsT=Gd.qT[:, m, :], rhs=Gd.kT[:, m, :], start=True, stop=True), reads=[Gd.kT, Gd.qT], writes=[pQK])
            S.op("act", lambda: A_.activation(out=f8(Gd.dec[:]), in_=pD[0:64, :], func=AF.Exp), reads=[pD], writes=[Gd.dec])
            B = Gd.B[0]; BT = Gd.BT[0]
            S.op("dve", lambda: V.tensor_tensor(out=B[:], in0=pKK[0:64, :].rearrange("p (m d) -> p m d", m=8), in1=Gd.nbeta[:, base:base + 8].unsqueeze(2).to_broadcast([64, 8, 64]), op=ALU.mult), reads=[pKK, Gd.nbeta], writes=[B])
            S.op("dve", lambda: V.tensor_tensor(out=B[:], in0=B[:], in1=Gd.dec[:], op=ALU.mult), reads=[B, Gd.dec], writes=[B])
            S.op("dve", lambda: V.tensor_tensor(out=B[:], in0=B[:], in1=C.strict01[0:64, 0:64].unsqueeze(1).to_broadcast([64, 8, 64]), op=ALU.mult), reads=[B, C.strict01], writes=[B])
            S.op("dve", lambda: V.tensor_tensor(out=f8(Gd.qk[:]), in0=pQK[0:64, :], in1=f8(Gd.dec[:]), op=ALU.mult), reads=[pQK, Gd.dec], writes=[Gd.qk])
            pbt, pqk = PS[0], PS[1]
            for m in range(8):
                S.op("pe", lambda m=m: nc.tensor.transpose(out=pbt[0:64, m * 64:(m + 1) * 64], in_=B[:, m, :], identity=I64), reads=[B, C.ident_f], writes=[pbt])
                S.op("pe", lambda m=m: nc.tensor.transpose(out=pqk[0:64, m * 64:(m + 1) * 64], in_=Gd.qk[:, m, :], identity=I64), reads=[Gd.qk, C.ident_f], writes=[pqk])
            S.op("act", lambda: A_.copy(out=f8(BT[:]), in_=pbt[0:64, :]), reads=[pbt], writes=[BT])
            S.op("dve", lambda: V.tensor_copy(out=f8(Gd.qkT[:, base:base + 8, :]), in_=pqk[0:64, :]), reads=[pqk], writes=[Gd.qkT])
            for lev in range(6):
                B = Gd.B[lev % 2]; BT = Gd.BT[lev % 2]; B2 = Gd.B[(lev + 1) % 2]; BT2 = Gd.BT[(lev + 1) % 2]
                pu = [PS[2], PS[3]]
                for m in range(8):
                    S.op("pe", lambda m=m, BT=BT: nc.tensor.matmul(pu[m // 4][0:64, (m % 4) * 128:(m % 4 + 1) * 128], lhsT=BT[:, m, :], rhs=Gd.U[:, base + m, :], start=True, stop=True), reads=[BT, Gd.U], writes=[pu[m // 4]])
                if lev < 5:
                    for m in range(8):
                        S.op("pe", lambda m=m, B=B, BT=BT: nc.tensor.matmul(PS[5][0:64, m * 64:(m + 1) * 64], lhsT=BT[:, m, :], rhs=B[:, m, :], start=True, stop=True), reads=[B, BT], writes=[PS[5]])
                        S.op("pe", lambda m=m, B=B, BT=BT: nc.tensor.matmul(PS[6][0:64, m * 64:(m + 1) * 64], lhsT=B[:, m, :], rhs=BT[:, m, :], start=True, stop=True), reads=[B, BT], writes=[PS[6]])
                for hh in range(2):
                    S.op("dve", lambda hh=hh: V.tensor_tensor(out=Gd.U[:, base + hh * 4:base + hh * 4 + 4, :], in0=Gd.U[:, base + hh * 4:base + hh * 4 + 4, :],
                                                              in1=pu[hh][0:64, :].rearrange("p (m d) -> p m d", m=4), op=ALU.add), reads=[pu[hh], Gd.U], writes=[Gd.U])
                if lev < 5:
                    S.op("act", lambda B2=B2: A_.copy(out=f8(B2[:]), in_=PS[5][0:64, :]), reads=[PS[5]], writes=[B2])
                    S.op("act", lambda BT2=BT2: A_.copy(out=f8(BT2[:]), in_=PS[6][0:64, :]), reads=[PS[6]], writes=[BT2])
            pw = PS[0]
            for m in range(8):
                S.op("pe", lambda m=m: nc.tensor.transpose(out=pw[0:64, m * 64:(m + 1) * 64], in_=Gd.U[:, base + m, 64:128], identity=I64), reads=[Gd.U, C.ident_f], writes=[pw])
            S.op("act", lambda: A_.copy(out=f8(Gd.wT[:, base:base + 8, :]), in_=pw[0:64, :]), reads=[pw], writes=[Gd.wT])
        for n in range(GC):
            p1, p2, p3 = PS[(n % 2) * 3], PS[(n % 2) * 3 + 1], PS[(n % 2) * 3 + 2]
            for h in range(4):
                nh = n * 4 + h
                S.op("pe", lambda h=h, nh=nh: nc.tensor.matmul(p1[0:64, h * 64:(h + 1) * 64], lhsT=Gd.wT[:, nh, :], rhs=Gd.S[:, h, :], start=True, stop=True), reads=[Gd.wT, Gd.S], writes=[p1])
            for h in range(4):
                nh = n * 4 + h
                S.op("pe", lambda h=h, nh=nh: nc.tensor.matmul(p2[0:64, h * 64:(h + 1) * 64], lhsT=Gd.qdT[:, nh, :], rhs=Gd.S[:, h, :], start=(h == 0), stop=False, skip_group_check=True), reads=[Gd.qdT, Gd.S], writes=[p2])
            S.op("dve", lambda n=n: V.tensor_tensor(out=Gd.vn[:].rearrange("p h d -> p (h d)"), in0=Gd.U[:, n * 4:(n + 1) * 4, 0:64].rearrange("p h d -> p h d"), in1=p1[0:64, 0:256], op=ALU.subtract) if False else
                 V.tensor_tensor(out=Gd.vn[:], in0=Gd.U[:, n * 4:(n + 1) * 4, 0:64], in1=p1[0:64, 0:256].rearrange("p (h d) -> p h d", h=4), op=ALU.subtract), reads=[Gd.U, p1], writes=[Gd.vn])
            for h in range(4):
                nh = n * 4 + h
                S.op("pe", lambda h=h, nh=nh: nc.tensor.matmul(p2[0:64, h * 64:(h + 1) * 64], lhsT=Gd.qkT[:, nh, :], rhs=Gd.vn[:, h, :], start=False, stop=True, skip_group_check=True), reads=[Gd.qkT, Gd.vn], writes=[p2])
            for h in range(4):
                nh = n * 4 + h
                S.op("pe", lambda h=h, nh=nh: nc.tensor.matmul(p3[0:64, h * 64:(h + 1) * 64], lhsT=Gd.kdec[:, nh, :], rhs=Gd.vn[:, h, :], start=True, stop=True), reads=[Gd.kdec, Gd.vn], writes=[p3])
            S.op("act", lambda n=n: A_.copy(out=Gd.o[:, n, :], in_=p2[0:64, 0:256]), reads=[p2], writes=[Gd.o])
            S.op("dve", lambda n=n: V.tensor_tensor(out=Gd.S[:], in0=Gd.S[:], in1=Gd.egl[:, n * 4:(n + 1) * 4].unsqueeze(2).to_broadcast([64, 4, 64]), op=ALU.mult), reads=[Gd.S, Gd.egl], writes=[Gd.S])
            S.op("dve", lambda: V.tensor_tensor(out=Gd.S[:], in0=Gd.S[:], in1=p3[0:64, 0:256].rearrange("p (h d) -> p h d", h=4), op=ALU.add), reads=[Gd.S, p3], writes=[Gd.S])
        o4 = lambda ap: ap.rearrange("p n (h d) -> p n h d", h=4)
        S.op("act", lambda: A_.activation(out=Gd.sz[:], in_=Gd.o[:], func=AF.Square), reads=[Gd.o], writes=[Gd.sz])
        S.op("dve", lambda: V.tensor_reduce(out=Gd.ms[:], in_=o4(Gd.sz[:]), axis=AX.X, op=ALU.add), reads=[Gd.sz], writes=[Gd.ms])
        S.op("dve", lambda: V.tensor_scalar(out=Gd.ms[:], in0=Gd.ms[:], scalar1=1.0 / 64, scalar2=1e-6, op0=ALU.mult, op1=ALU.add), reads=[Gd.ms], writes=[Gd.ms])
        S.op("act", lambda: A_.activation(out=Gd.ms[:], in_=Gd.ms[:], func=AF.Sqrt), reads=[Gd.ms], writes=[Gd.ms])
        S.op("dve", lambda: V.reciprocal(out=Gd.ms[:], in_=Gd.ms[:]), reads=[Gd.ms], writes=[Gd.ms])
        S.op("dve", lambda: V.tensor_tensor(out=o4(Gd.o[:]), in0=o4(Gd.o[:]), in1=Gd.ms[:].unsqueeze(3).to_broadcast([64, GC, 4, 64]), op=ALU.mult), reads=[Gd.o, Gd.ms], writes=[Gd.o])
        S.op("dve", lambda: V.tensor_tensor(out=Gd.o[:].rearrange("p n (h d) -> p (n h) d", h=4), in0=Gd.o[:].rearrange("p n (h d) -> p (n h) d", h=4), in1=Gd.nw[:].unsqueeze(1).to_broadcast([64, NHG, 64]), op=ALU.mult), reads=[Gd.o, Gd.nw], writes=[Gd.o])
        S.op("act", lambda: A_.activation(out=Gd.sz[:], in_=Gd.zba[:, :, 0:256], func=AF.Silu), reads=[Gd.zba], writes=[Gd.sz])
        S.op("dve", lambda: V.tensor_tensor(out=Gd.y[:], in0=Gd.o[:], in1=Gd.sz[:], op=ALU.mult), reads=[Gd.o, Gd.sz], writes=[Gd.y])
        for j in range(2):
            pyb = PS[6 + j]
            pyv = pyb[:].bitcast(BF16)
            for n in range(GC):
                S.op("pe", lambda n=n, j=j, pyv=pyv: nc.tensor.transpose(out=pyv[:, n * 64:(n + 1) * 64], in_=Gd.y[:, n, j * 128:(j + 1) * 128], identity=C.ident_b[0:64, 0:64]), reads=[Gd.y, C.ident_b], writes=[pyb])
            S.op("act", lambda j=j, pyv=pyv: A_.copy(out=Gd.ymT[:, j, :], in_=pyv[:, 0:512]), reads=[pyb], writes=[Gd.ymT])
            S.dma("sp", Z.mixT[4 + j][:, t0:t0 + 512], Gd.ymT[:, j, :], reads=[Gd.ymT], writes=[Z.mixT])


def nsa_consts():
    c = {}
    t = np.arange(SEQ)
    c["expand"] = (t[None, :] // 64 == np.arange(64)[:, None]).astype(np.float32)
    sb = np.zeros((NT, 128, 64), np.float32)
    j = np.arange(64)[None, :]
    for qb in range(NT):
        cur = ((qb * 128 + np.arange(128)) // 64)[:, None]
        forced = (j == 0) | (j == cur) | (j == cur - 1)
        sb[qb] = np.where(j <= cur, np.where(forced, 1.0e4, 0.0), -1.0e30)
    c["selbias"] = sb
    c0 = np.arange(256)[:, None] * 16; s0 = np.arange(64)[None, :] * 64
    ov = np.minimum(c0 + 32, s0 + 64) - np.maximum(c0, s0)
    ov = (np.maximum(ov, 0) / 16).astype(np.float32); ov[255] = 0
    c["ov"] = ov
    return c


def alloc_nsa(C):
    N = Ctx(); sb = C.sb
    N.QT = sb("n_QT", [128, 4, SEQ], BF16)
    N.KsT = sb("n_KsT", [128, SEQ], BF16); N.KwT = sb("n_KwT", [128, SEQ], BF16)
    N.XcT = [sb(f"n_XcT{i}", [128, SEQ], BF16) for i in range(2)]
    N.Vs = sb("n_Vs", [128, NT, 2, 65], BF16); N.Vw = sb("n_Vw", [128, NT, 2, 65], BF16)
    N.gates = sb("n_gates", [128, NT, 24], F32)
    N.w1s = sb("n_w1s", [128, 32, 128], BF16)
    N.pe = sb("n_pe", [32, 64], F32); N.peT = sb("n_peT", [64, 32], BF16)
    N.bias = sb("n_bias", [128, 1], F32)
    N.w2f = sb("n_w2f", [128, 64], F32)
    N.w2p = sb("n_w2p", [128, 2, 128], BF16); N.w2v = sb("n_w2v", [128, 64], BF16)
    N.hT = [sb(f"n_hT{i}", [128, 256], BF16) for i in range(2)]
    N.kcT = sb("n_kcT", [128, 256], BF16)
    N.vcx = sb("n_vcx", [128, 2, 2, 65], BF16)
    N.ovb = sb("n_ovb", [128, 2, 64], BF16)
    N.Ex = sb("n_Ex", [64, SEQ], BF16)
    N.e = [sb(f"n_e{i}", [128, 512], BF16) for i in range(2)]
    N.selb = sb("n_selb", [128, 64], F32)
    N.rz = sb("n_rz", [128, 3, 4], F32)
    N.imp = sb("n_imp", [128, 64], F32); N.imp2 = sb("n_imp2", [128, 64], F32)
    N.m8 = sb("n_m8", [128, 8], F32); N.m8b = sb("n_m8b", [128, 8], F32)
    N.msk = sb("n_msk", [128, 64], F32); N.msk2 = sb("n_msk2", [128, 64], F32)
    N.mT = sb("n_mT", [64, 128], BF16)
    N.coef = sb("n_coef", [128, 3, 4], F32)
    N.o = sb("n_o", [128, 8, 64], F32)
    N.ob = sb("n_ob", [128, 512], BF16)
    N.omT = sb("n_omT", [128, 4, 512], BF16)
    return N


def emit_nsa(C, N, Z, W, l):
    S = C.S; nc = C.nc; PS = C.PS; V = nc.vector; A_ = nc.scalar
    S.dma("sp", N.QT[:], Z.QT[:].rearrange("g p t -> p g t"), reads=[Z.QT], writes=[N.QT])
    S.dma("sp", N.KsT[:], Z.KsT[:], reads=[Z.KsT], writes=[N.KsT])
    S.dma("sp", N.KwT[:], Z.KwT[:], reads=[Z.KwT], writes=[N.KwT])
    S.dma("sp", N.XcT[0][:], Z.KcT[:], reads=[Z.KcT], writes=[N.XcT[0]])
    S.dma("sp", N.XcT[1][:], Z.VcT[:], reads=[Z.VcT], writes=[N.XcT[1]])
    tokb = Z.tok[:].bitcast(BF16)
    for h in range(2):
        S.dma("sp", N.Vs[:, :, h, 0:64], tokb[:, 1024 + h * 64:1024 + (h + 1) * 64].rearrange("(t p) d -> p t d", p=128), reads=[Z.tok], writes=[N.Vs])
        S.dma("sp", N.Vw[:, :, h, 0:64], tokb[:, 1152 + h * 64:1152 + (h + 1) * 64].rearrange("(t p) d -> p t d", p=128), reads=[Z.tok], writes=[N.Vw])
    S.op("pool", lambda: nc.gpsimd.memset(N.Vs[:, :, :, 64:65], 1.0), reads=[], writes=[N.Vs])
    S.op("pool", lambda: nc.gpsimd.memset(N.Vw[:, :, :, 64:65], 1.0), reads=[], writes=[N.Vw])
    S.dma("sp", N.gates[:], Z.tok[:, 264:288].rearrange("(t p) c -> p t c", p=128), reads=[Z.tok], writes=[N.gates])
    for h0 in range(0, SEQ, 1024):
        S.dma("pool", N.Ex[:, h0:h0 + 1024], C.cin["expand"][:, h0:h0 + 1024], reads=[C.cin["expand"]], writes=[N.Ex])
    S.dma("pool", N.ovb[:], C.cin["ov"][:].rearrange("(c p) j -> p c j", p=128), reads=[C.cin["ov"]], writes=[N.ovb])
    S.op("pool", lambda: nc.gpsimd.memset(N.vcx[:, :, :, 64:65], 1.0), reads=[], writes=[N.vcx])
    for kv in range(2):
        X = N.XcT[kv]
        for half in range(2):
            S.dma("pool", N.w1s[half * 64:(half + 1) * 64, :, :], W["nsa_cmp_w1"][l, kv].rearrange("(j d) h -> d j h", d=64), reads=[W["nsa_cmp_w1"]], writes=[N.w1s])
        S.dma("sp", N.pe[:], W["nsa_cmp_pe"][l, kv], reads=[W["nsa_cmp_pe"]], writes=[N.pe])
        S.op("pe", lambda: nc.tensor.transpose(out=PS[7][0:64, 0:32], in_=N.pe[:], identity=C.ident_f[0:32, 0:32]), reads=[N.pe, C.ident_f], writes=[PS[7]])
        S.op("act", lambda: A_.copy(out=N.peT[:], in_=PS[7][0:64, 0:32]), reads=[PS[7]], writes=[N.peT])
        for j in range(32):
            S.op("pe", lambda j=j: nc.tensor.matmul(PS[6][:, 0:1], lhsT=N.w1s[0:64, j, :], rhs=N.peT[:, j:j + 1], start=(j == 0), stop=(j == 31)), reads=[N.w1s, N.peT], writes=[PS[6]])
        S.op("act", lambda: A_.copy(out=N.bias[:], in_=PS[6][:, 0:1]), reads=[PS[6]], writes=[N.bias])
        S.dma("sp", N.w2f[:], W["nsa_cmp_w2"][l, kv], reads=[W["nsa_cmp_w2"]], writes=[N.w2f])
        if kv == 0:
            S.op("dve", lambda: V.memset(N.w2p[:], 0.0), writes=[N.w2p])
            S.op("dve", lambda: V.tensor_copy(out=N.w2p[:, 0, 0:64], in_=N.w2f[:]), reads=[N.w2f, N.w2p], writes=[N.w2p])
            S.op("dve", lambda: V.tensor_copy(out=N.w2p[:, 1, 64:128], in_=N.w2f[:]), reads=[N.w2f, N.w2p], writes=[N.w2p])
        else:
            S.op("dve", lambda: V.tensor_copy(out=N.w2v[:], in_=N.w2f[:]), reads=[N.w2f], writes=[N.w2v])
        for h in range(2):
            ph = PS[h]
            for j in range(32):
                S.op("pe", lambda j=j, h=h, ph=ph: nc.tensor.matmul(ph[:, 0:255], lhsT=N.w1s[h * 64:(h + 1) * 64, j, :], rhs=X[h * 64:(h + 1) * 64, j:j + 16 * 254 + 1:16], start=(j == 0), stop=(j == 31)),
                     reads=[N.w1s, X], writes=[ph])
            S.op("dve", lambda h=h: V.memset(N.hT[h][:, 255:256], 0.0), writes=[N.hT[h]])
            S.op("act", lambda h=h, ph=ph: A_.activation(out=N.hT[h][:, 0:255], in_=ph[:, 0:255], func=AF.Silu, bias=N.bias[:]), reads=[ph, N.bias, N.hT[h]], writes=[N.hT[h]])
        if kv == 0:
            for h in range(2):
                S.op("pe", lambda h=h: nc.tensor.matmul(PS[2][:, 0:256], lhsT=N.w2p[:, h, :], rhs=N.hT[h][:], start=(h == 0), stop=(h == 1)), reads=[N.w2p, N.hT[h]], writes=[PS[2]])
            S.op("act", lambda: A_.copy(out=N.kcT[:], in_=PS[2][:, 0:256]), reads=[PS[2]], writes=[N.kcT])
        else:
            for h in range(2):
                for cc in range(2):
                    S.op("pe", lambda h=h, cc=cc: nc.tensor.matmul(PS[3][:, (h * 2 + cc) * 64:(h * 2 + cc + 1) * 64], lhsT=N.hT[h][:, cc * 128:(cc + 1) * 128], rhs=N.w2v[:], start=True, stop=True),
                         reads=[N.hT[h], N.w2v], writes=[PS[3]])
            S.op("act", lambda: A_.copy(out=N.vcx[:, :, :, 0:64].rearrange("p c h d -> p h c d"), in_=PS[3][:, 0:256].rearrange("p (h c d) -> p h c d", h=2, c=2)), reads=[PS[3], N.vcx], writes=[N.vcx])
    kctr = [0]

    def chunk(KT, kcol, Vrhs, vb, acc, first, last, qb, kvh, mask, extra=None):
        k = kctr[0]; kctr[0] += 1
        ps = PS[k % 2]; e = N.e[k % 2]
        hs = slice(kvh * 64, (kvh + 1) * 64)
        S.op("pe", lambda: nc.tensor.matmul(ps[:], lhsT=KT[hs, kcol], rhs=N.QT[hs, :, qb * 128:(qb + 1) * 128], start=True, stop=True), reads=[KT, N.QT], writes=[ps])
        S.op("act", lambda: A_.activation(out=e[:], in_=ps[:], func=AF.Exp, scale=0.125), reads=[ps], writes=[e])
        e3 = e[:].rearrange("p (g q) -> p g q", g=4)
        if mask is not None and mask[0] == "aff":
            _, cm, base, qstep = mask
            S.op("pool", lambda: nc.gpsimd.affine_select(out=e3, in_=e3, pattern=[[0, 4], [qstep, 128]], compare_op=ALU.is_ge, fill=0.0, base=base, channel_multiplier=cm), reads=[e], writes=[e])
        elif mask is not None and mask[0] == "blk":
            kc = mask[1]; pm = PS[2 + k % 2]
            S.op("pe", lambda: nc.tensor.matmul(pm[:, 0:128], lhsT=N.Ex[:, kc * 128:(kc + 1) * 128], rhs=N.mT[:], start=True, stop=True), reads=[N.Ex, N.mT], writes=[pm])
            S.op("dve", lambda: V.tensor_tensor(out=e3, in0=e3, in1=pm[:, 0:128].unsqueeze(1).to_broadcast([128, 4, 128]), op=ALU.mult), reads=[e, pm], writes=[e])
        for g in range(4):
            S.op("pe", lambda g=g: nc.tensor.matmul(acc[:, g * 65:(g + 1) * 65], lhsT=e[:, g * 128:(g + 1) * 128], rhs=Vrhs, start=(first and g == 0), stop=(last and g == 3), skip_group_check=True),
                 reads=[e, vb], writes=[acc])
            if extra is not None:
                pi, rhs2, b2 = extra
                S.op("pe", lambda g=g: nc.tensor.matmul(pi[:, g * 64:(g + 1) * 64], lhsT=e[:, g * 128:(g + 1) * 128], rhs=rhs2, start=(first and g == 0), stop=(last and g == 3), skip_group_check=True),
                     reads=[e, b2], writes=[pi])

    def finish_branch(acc, r):
        a3 = acc[:, 0:260].rearrange("p (g d) -> p g d", g=4)
        S.op("dve", lambda: V.tensor_scalar(out=N.rz[:, r, :], in0=a3[:, :, 64], scalar1=1e-30, scalar2=None, op0=ALU.max), reads=[acc, N.rz], writes=[N.rz])
        S.op("dve", lambda: V.reciprocal(out=N.rz[:, r, :], in_=N.rz[:, r, :]), reads=[N.rz], writes=[N.rz])

    def combine(acc, r, qb, kvh, first):
        a3 = acc[:, 0:260].rearrange("p (g d) -> p g d", g=4)
        gsl = N.gates[:, qb, :].rearrange("p (h r) -> p h r", r=3)[:, kvh * 4:(kvh + 1) * 4, r]
        S.op("dve", lambda: V.tensor_tensor(out=N.coef[:, r, :], in0=N.rz[:, r, :], in1=gsl, op=ALU.mult), reads=[N.rz, N.gates, N.coef], writes=[N.coef])
        for g in range(4):
            h = kvh * 4 + g
            if first:
                S.op("dve", lambda g=g, h=h: V.tensor_scalar(out=N.o[:, h, :], in0=a3[:, g, 0:64], scalar1=N.coef[:, r, g:g + 1], scalar2=None, op0=ALU.mult), reads=[acc, N.coef, N.o], writes=[N.o])
            else:
                S.op("dve", lambda g=g, h=h: V.scalar_tensor_tensor(out=N.o[:, h, :], in0=a3[:, g, 0:64], scalar=N.coef[:, r, g:g + 1], in1=N.o[:, h, :], op0=ALU.mult, op1=ALU.add), reads=[acc, N.coef, N.o], writes=[N.o])

    for qb in range(NT):
        S.dma("sp", N.selb[:], C.cin["selbias"][qb], reads=[C.cin["selbias"]], writes=[N.selb])
        for kvh in range(2):
            accC = PS[4]; pimp = PS[5]
            ncc = 1 if qb < 16 else 2
            for cc in range(ncc):
                base = -(16 * (128 * cc - 8 * qb) + 31)
                chunk(N.kcT, slice(cc * 128, (cc + 1) * 128), N.vcx[:, cc, kvh, :], N.vcx, accC, cc == 0, cc == ncc - 1, qb, kvh, ("aff", -16, base, 1), extra=(pimp, N.ovb[:, cc, :], N.ovb))
            finish_branch(accC, 0)
            for g in range(4):
                if g == 0:
                    S.op("dve", lambda: V.tensor_scalar(out=N.imp[:], in0=pimp[:, 0:64], scalar1=N.rz[:, 0, 0:1], scalar2=None, op0=ALU.mult), reads=[pimp, N.rz], writes=[N.imp])
                else:
                    S.op("dve", lambda g=g: V.scalar_tensor_tensor(out=N.imp[:], in0=pimp[:, g * 64:(g + 1) * 64], scalar=N.rz[:, 0, g:g + 1], in1=N.imp[:], op0=ALU.mult, op1=ALU.add), reads=[pimp, N.rz, N.imp], writes=[N.imp])
            combine(accC, 0, qb, kvh, True)
            S.op("dve", lambda: V.tensor_tensor(out=N.imp[:], in0=N.imp[:], in1=N.selb[:], op=ALU.add), reads=[N.imp, N.selb], writes=[N.imp])
            S.op("dve", lambda: V.max(out=N.m8[:], in_=N.imp[:]), reads=[N.imp], writes=[N.m8])
            S.op("dve", lambda: V.match_replace(out=N.imp2[:], in_to_replace=N.m8[:], in_values=N.imp[:], imm_value=-3.0e38), reads=[N.imp, N.m8], writes=[N.imp2])
            S.op("dve", lambda: V.max(out=N.m8b[:], in_=N.imp2[:]), reads=[N.imp2], writes=[N.m8b])
            S.op("dve", lambda: V.tensor_scalar(out=N.msk[:], in0=N.imp[:], scalar1=N.m8b[:, 7:8], scalar2=None, op0=ALU.is_ge), reads=[N.imp, N.m8b], writes=[N.msk])
            S.op("dve", lambda: V.tensor_scalar(out=N.msk2[:], in0=N.imp[:], scalar1=-1.0e29, scalar2=None, op0=ALU.is_gt), reads=[N.imp], writes=[N.msk2])
            S.op("dve", lambda: V.tensor_tensor(out=N.msk[:], in0=N.msk[:], in1=N.msk2[:], op=ALU.mult), reads=[N.msk, N.msk2], writes=[N.msk])
            S.op("pe", lambda: nc.tensor.transpose(out=PS[7][0:64, 0:128], in_=N.msk[:], identity=C.ident_f[:]), reads=[N.msk, C.ident_f], writes=[PS[7]])
            S.op("act", lambda: A_.copy(out=N.mT[:], in_=PS[7][0:64, 0:128]), reads=[PS[7]], writes=[N.mT])
            accS = PS[6]
            for kc in range(qb + 1):
                mask = ("aff", -1, 0, 1) if kc == qb else ("blk", kc)
                chunk(N.KsT, slice(kc * 128, (kc + 1) * 128), N.Vs[:, kc, kvh, :], N.Vs, accS, kc == 0, kc == qb, qb, kvh, mask)
            finish_branch(accS, 1)
            combine(accS, 1, qb, kvh, False)
            accW = PS[4]
            k0 = max(0, qb - 4)
            for kc in range(k0, qb + 1):
                mask = ("aff", -1, 0, 1) if kc == qb else (("aff", 1, -1, -1) if kc == qb - 4 else None)
                chunk(N.KwT, slice(kc * 128, (kc + 1) * 128), N.Vw[:, kc, kvh, :], N.Vw, accW, kc == k0, kc == qb, qb, kvh, mask)
            finish_branch(accW, 2)
            combine(accW, 2, qb, kvh, False)
        S.op("act", lambda: A_.copy(out=N.ob[:], in_=N.o[:].rearrange("p h d -> p (h d)")), reads=[N.o], writes=[N.ob])
        pov = PS[7][:].bitcast(BF16)
        for c in range(4):
            S.op("pe", lambda c=c: nc.tensor.transpose(out=pov[:, c * 128:(c + 1) * 128], in_=N.ob[:, c * 128:(c + 1) * 128], identity=C.ident_b[:]), reads=[N.ob, C.ident_b], writes=[PS[7]])
        S.op("dve", lambda: V.tensor_copy(out=N.omT[:, :, (qb % 4) * 128:(qb % 4 + 1) * 128], in_=pov[:, 0:512].rearrange("p (c t) -> p c t", c=4)), reads=[PS[7], N.omT], writes=[N.omT])
        if qb % 4 == 3:
            q0 = (qb // 4) * 512
            S.dma("sp", Z.mixT[0:4, :, q0:q0 + 512].rearrange("c p t -> p c t"), N.omT[:], reads=[N.omT], writes=[Z.mixT])


WNAMES = {"w_in_p": [DEPTH, D, NCOLP], "w_out": [DEPTH, D, D], "nsa_cmp_pe": [DEPTH, 2, 32, 64], "nsa_cmp_w1": [DEPTH, 2, 2048, 128],
          "nsa_cmp_w2": [DEPTH, 2, 128, 64], "gdn_conv_w": [DEPTH, 4, 768], "gdn_a_log": [DEPTH, 4], "gdn_dt_bias": [DEPTH, 4],
          "gdn_norm_w": [DEPTH, 64], "conf_dw_w": [DEPTH, 31, 256], "conf_dw_b": [DEPTH, 256], "conf_ln_w": [DEPTH, 256], "conf_ln_b": [DEPTH, 256],
          "ln1_w": [DEPTH, D], "ln1_b": [DEPTH, D], "ln2_w": [DEPTH, D], "ln2_b": [DEPTH, D], "moe_w_group": [DEPTH, D, 4], "moe_b_group": [DEPTH, 4],
          "moe_w_expert": [DEPTH, D, NE], "moe_b_expert": [DEPTH, NE], "moe_w_gate": [DEPTH, NE, D, FF], "moe_w_up": [DEPTH, NE, D, FF],
          "moe_w_down": [DEPTH, NE, FF, D]}


STAGE_W = {"proj": ("w_in_p",), "conf": ("conf_",), "gdn": ("gdn_",), "nsa": ("nsa_",), "outp": ("w_out", "ln1_"), "moe": ("moe_", "ln2_")}


def all_consts():
    c = host_consts()
    c.update(nsa_consts())
    return c


def build_program(depth=DEPTH, stages=("proj", "conf", "gdn", "nsa", "outp", "moe"), debug=False):
    nc = bass.Bass("TRN2", target_bir_lowering=False)
    S = Sched(nc); C = Ctx()
    setup(nc, S, C)
    W = {k: C.dram(k, [depth] + v[1:], F32, "ExternalInput") for k, v in WNAMES.items() if any(k.startswith(p) for st_ in stages for p in STAGE_W[st_])}
    xin = C.dram("x", [SEQ, D], F32, "ExternalInput")
    yout = C.dram("y", [SEQ, D], F32, "ExternalOutput")
    Z = alloc_scratch(C, "ExternalOutput")
    C.open_arena()
    base = C.off
    st = {}
    for nm, fn in (("proj", alloc_proj), ("conf", alloc_conf), ("gdn", lambda C: alloc_gdn(C, Z)), ("nsa", alloc_nsa), ("outp", alloc_outp), ("moe", alloc_moe)):
        C.off = base
        st[nm] = fn(C)

    def emit():
        emit_consts(C)
        xs = [xin] + [Z.xa, Z.xb] * depth
        import os as _os
        _ls = [int(v) for v in _os.environ.get("LAYERS", "").split(",") if v] or list(range(depth))
        for li, l in enumerate(_ls):
            x = xs[li]; xo = yout if li == len(_ls) - 1 else xs[li + 1]
            if "proj" in stages:
                emit_proj(C, st["proj"], Z, W, l, x); S.barrier()
            if "conf" in stages:
                emit_conf(C, st["conf"], Z, W, l); S.barrier()
            if "gdn" in stages:
                emit_gdn(C, st["gdn"], Z, W, l); S.barrier()
            if "nsa" in stages:
                emit_nsa(C, st["nsa"], Z, W, l); S.barrier()
            if "outp" in stages:
                emit_outp(C, st["outp"], Z, W, l, x, Z.x1); S.barrier()
            if "moe" in stages:
                emit_moe(C, st["moe"], W, l, Z.x1, xo); S.barrier()
        S.finish("sp", [yout] + ([Z.mixT, Z.x1] if debug else []))
    S.start_pass(True); emit(); S.start_pass(False); emit()
    return nc, S


_PERM = None


def make_inputs(inputs, depth=DEPTH, stages=("proj", "conf", "gdn", "nsa", "outp", "moe")):
    global _PERM
    if _PERM is None:
        _PERM = w_in_perm()
    f = lambda a: np.ascontiguousarray(np.asarray(a, dtype=np.float32))
    shared = {}
    for k in WNAMES:
        if not any(k.startswith(p) for st_ in stages for p in STAGE_W[st_]):
            continue
        if k == "w_in_p":
            shared[k] = np.ascontiguousarray(f(inputs["w_in"])[:depth][:, :, _PERM])
        else:
            shared[k] = f(inputs[k])[:depth]
    for k, v in all_consts().items():
        shared["c_" + k] = v
    x = f(inputs["x"])
    return [dict(x=x[b], **shared) for b in range(8)]


_NC = None
FUSED_DEPTH = DEPTH


def kernel(**inputs):
    global _NC
    if _NC is None:
        _NC = build_program(depth=FUSED_DEPTH)[0]
    x = np.ascontiguousarray(np.asarray(inputs["x"], dtype=np.float32))
    for l0 in range(0, DEPTH, FUSED_DEPTH):
        sub = {k: (v if k == "x" else np.asarray(v)[l0:l0 + FUSED_DEPTH]) for k, v in inputs.items()}
        sub["x"] = x
        in_maps = make_inputs(sub, depth=FUSED_DEPTH)
        res = run_bass_kernel_spmd(_NC, in_maps, core_ids=list(range(8)))
        x = np.stack([np.asarray(r["y"], dtype=np.float32) for r in res.results], axis=0)
    return x
```
